# Optimizing a Trainium2 kernel written in Bass

```python
import math
import jax, jax.numpy as jnp
from jax import lax
import numpy as np

D_MODEL = 1024
BATCH = 4
SEQ = 4096
DEPTH = 4

RET_HEADS = 8
RET_DK = 64
RET_DV = 128
RET_CHUNK = 128
CONV_W = 1024
CONV_K = 3
MLA_HEADS = 8
MLA_NOPE = 128
MLA_ROPE = 64
MLA_DV = 128
Q_LORA = 384
KV_LORA = 256
ATTN_BLOCK = 128
ROPE_BASE = 10000.0
EPS = 1e-6
N_EVEN = (DEPTH + 1) // 2
N_ODD = DEPTH // 2

RET_QK = RET_HEADS * RET_DK
RET_V = RET_HEADS * RET_DV
EVEN_SPLITS = (RET_QK, RET_QK, RET_V, RET_V, CONV_W, CONV_W, CONV_W, CONV_W)
EVEN_IN = sum(EVEN_SPLITS)
EVEN_MIX = RET_V + CONV_W
MLA_QK = MLA_NOPE + MLA_ROPE
MLA_V = MLA_HEADS * MLA_DV
ODD_SPLITS = (Q_LORA, KV_LORA, MLA_ROPE, MLA_V)
ODD_IN = sum(ODD_SPLITS)

kernel_name = "hybrid_retnet_shortconv_mla_trunk"


def rms_norm(x, g):
    xf = x.astype(jnp.float32)
    y = xf * lax.rsqrt(jnp.mean(xf * xf, axis=-1, keepdims=True) + EPS)
    return (y * g.astype(jnp.float32)).astype(x.dtype)


def rope(x, positions):
    d = x.shape[-1]
    inv = ROPE_BASE ** (-jnp.arange(0, d, 2, dtype=jnp.float32) / d)
    ang = positions.astype(jnp.float32)[..., None] * inv
    cos = jnp.cos(ang)[:, :, None, :]
    sin = jnp.sin(ang)[:, :, None, :]
    xf = x.astype(jnp.float32)
    x1, x2 = xf[..., : d // 2], xf[..., d // 2:]
    out = jnp.concatenate([x1 * cos - x2 * sin, x1 * sin + x2 * cos], axis=-1)
    return out.astype(x.dtype)


def split_cols(h, sizes):
    return jnp.split(h, np.cumsum(sizes)[:-1].tolist(), axis=-1)


def retention(q, k, v):
    b, s, h, dk = q.shape
    dv = v.shape[-1]
    c = RET_CHUNK
    n = s // c
    dt = q.dtype
    log_gamma = jnp.log1p(-jnp.exp2(-5.0 - jnp.arange(h, dtype=jnp.float32)))
    i = jnp.arange(c, dtype=jnp.float32)
    rel = i[:, None] - i[None, :]
    intra = jnp.where(rel >= 0, jnp.exp(log_gamma[:, None, None] * jnp.maximum(rel, 0.0)), 0.0).astype(dt)
    xi = jnp.exp(log_gamma[:, None] * (i + 1.0)).astype(dt)
    zeta = jnp.exp(log_gamma[:, None] * (c - 1.0 - i)).astype(dt)
    gamma_c = jnp.exp(log_gamma * c).astype(dt)
    qc = q.reshape(b, n, c, h, dk)
    kc = k.reshape(b, n, c, h, dk)
    vc = v.reshape(b, n, c, h, dv)
    scores = jnp.einsum('bnihd,bnjhd->bnhij', qc, kc) * intra
    inner = jnp.einsum('bnhij,bnjhe->bnihe', scores, vc)
    kv = jnp.einsum('bnjhd,hj,bnjhe->nbhde', kc, zeta, vc)

    def step(state, kv_i):
        return gamma_c[None, :, None, None] * state + kv_i, state

    _, prev = lax.scan(step, jnp.zeros((b, h, dk, dv), dt), kv)
    cross = jnp.einsum('bnihd,nbhde,hi->bnihe', qc, prev, xi)
    return (inner + cross).reshape(b, s, h, dv)


def short_conv(u, w, bias):
    s = u.shape[1]
    up = jnp.pad(u, ((0, 0), (CONV_K - 1, 0), (0, 0)))
    out = up[:, 0:s] * w[0]
    for tap in range(1, CONV_K):
        out = out + up[:, tap:tap + s] * w[tap]
    return out + bias


def even_layer(h, positions, w_in, conv_w, conv_b, gn, w_out):
    b, s, _ = h.shape
    q, k, v, g_ret, cb, cc, cx, g_conv = split_cols(h @ w_in, EVEN_SPLITS)
    q = rope(q.reshape(b, s, RET_HEADS, RET_DK), positions)
    k = rope(k.reshape(b, s, RET_HEADS, RET_DK), positions) * (RET_DK ** -0.5)
    v = v.reshape(b, s, RET_HEADS, RET_DV)
    r = retention(q, k, v)
    r = rms_norm(r, gn.reshape(RET_HEADS, RET_DV)).reshape(b, s, RET_V)
    y_conv = cb * short_conv(cc * cx, conv_w, conv_b)
    mix = jnp.concatenate([r * jax.nn.silu(g_ret), y_conv * jax.nn.silu(g_conv)], axis=-1)
    return mix @ w_out


def causal_mla_attention(q_nope, q_rope, k_nope, k_rope, v):
    s = q_nope.shape[1]
    scale = MLA_QK ** -0.5
    outs = []
    for blk in range(s // ATTN_BLOCK):
        lo, hi = blk * ATTN_BLOCK, (blk + 1) * ATTN_BLOCK
        sc = (jnp.einsum('bqhd,bkhd->bhqk', q_nope[:, lo:hi].astype(jnp.float32), k_nope[:, :hi].astype(jnp.float32))
              + jnp.einsum('bqhd,bkd->bhqk', q_rope[:, lo:hi].astype(jnp.float32), k_rope[:, :hi].astype(jnp.float32))) * scale
        qpos = lo + jnp.arange(ATTN_BLOCK)
        kpos = jnp.arange(hi)
        sc = jnp.where(kpos[None, :] <= qpos[:, None], sc, -jnp.inf)
        p = jax.nn.softmax(sc, axis=-1).astype(v.dtype)
        outs.append(jnp.einsum('bhqk,bkhd->bqhd', p, v[:, :hi]))
    return jnp.concatenate(outs, axis=1)


def odd_layer(h, positions, w_in, q_a_norm, w_qb, kv_a_norm, w_kvb, w_out):
    b, s, _ = h.shape
    cq, ckv, k_rope, gate = split_cols(h @ w_in, ODD_SPLITS)
    q = (rms_norm(cq, q_a_norm) @ w_qb).reshape(b, s, MLA_HEADS, MLA_QK)
    q_nope = q[..., :MLA_NOPE]
    q_rope = rope(q[..., MLA_NOPE:], positions)
    kv = (rms_norm(ckv, kv_a_norm) @ w_kvb).reshape(b, s, MLA_HEADS, MLA_NOPE + MLA_DV)
    k_nope, v = kv[..., :MLA_NOPE], kv[..., MLA_NOPE:]
    k_rope = rope(k_rope[:, :, None, :], positions)[:, :, 0]
    o = causal_mla_attention(q_nope, q_rope, k_nope, k_rope, v)
    return (o.reshape(b, s, MLA_V) * jax.nn.silu(gate)) @ w_out


def setup_inputs(seed: int = 0) -> dict:
    key = jax.random.key(seed)
    ks = jax.random.split(key, 16)
    f32 = jnp.float32
    nrm = lambda k, shape, scale: jax.random.normal(k, shape, f32) * scale
    x = jax.random.normal(ks[0], (BATCH, SEQ, D_MODEL), f32)
    offsets = jax.random.randint(ks[1], (BATCH, 1), 0, 1024, dtype=jnp.int32)
    positions = (offsets + jnp.arange(SEQ, dtype=jnp.int32)[None, :]).astype(jnp.int32)
    pre_norm = 1.0 + nrm(ks[2], (DEPTH, D_MODEL), 0.02)
    post_norm = 1.0 + nrm(ks[3], (DEPTH, D_MODEL), 0.02)
    even_w_in = nrm(ks[4], (N_EVEN, D_MODEL, EVEN_IN), D_MODEL ** -0.5)
    even_conv_w = nrm(ks[5], (N_EVEN, CONV_K, CONV_W), CONV_K ** -0.5)
    even_conv_b = nrm(ks[6], (N_EVEN, CONV_W), 0.01)
    ret_gn = 1.0 + nrm(ks[7], (N_EVEN, RET_V), 0.02)
    even_w_out = nrm(ks[8], (N_EVEN, EVEN_MIX, D_MODEL), EVEN_MIX ** -0.5)
    odd_w_in = nrm(ks[9], (N_ODD, D_MODEL, ODD_IN), D_MODEL ** -0.5)
    q_a_norm = 1.0 + nrm(ks[10], (N_ODD, Q_LORA), 0.02)
    w_qb = nrm(ks[11], (N_ODD, Q_LORA, MLA_HEADS * MLA_QK), Q_LORA ** -0.5)
    kv_a_norm = 1.0 + nrm(ks[12], (N_ODD, KV_LORA), 0.02)
    w_kvb = nrm(ks[13], (N_ODD, KV_LORA, MLA_HEADS * (MLA_NOPE + MLA_DV)), KV_LORA ** -0.5)
    odd_w_out = nrm(ks[14], (N_ODD, MLA_V, D_MODEL), MLA_V ** -0.5)
    return {"x": x, "positions": positions, "pre_norm": pre_norm, "post_norm": post_norm,
            "even_w_in": even_w_in, "even_conv_w": even_conv_w, "even_conv_b": even_conv_b,
            "ret_gn": ret_gn, "even_w_out": even_w_out, "odd_w_in": odd_w_in,
            "q_a_norm": q_a_norm, "w_qb": w_qb, "kv_a_norm": kv_a_norm, "w_kvb": w_kvb,
            "odd_w_out": odd_w_out}


def reference(x, positions, pre_norm, post_norm, even_w_in, even_conv_w, even_conv_b, ret_gn,
              even_w_out, odd_w_in, q_a_norm, w_qb, kv_a_norm, w_kvb, odd_w_out):
    h = x
    for layer in range(DEPTH):
        u = rms_norm(h, pre_norm[layer])
        if layer % 2 == 0:
            e = layer // 2
            y = even_layer(u, positions, even_w_in[e], even_conv_w[e], even_conv_b[e], ret_gn[e], even_w_out[e])
        else:
            o = layer // 2
            y = odd_layer(u, positions, odd_w_in[o], q_a_norm[o], w_qb[o], kv_a_norm[o], w_kvb[o], odd_w_out[o])
        h = h + rms_norm(y, post_norm[layer])
    return h
```

```python
import math
import numpy as np
import concourse.bass as bass
import concourse.mybir as mybir
from concourse.bass_utils import run_bass_kernel_spmd

F32 = mybir.dt.float32
BF16 = mybir.dt.bfloat16
I32 = mybir.dt.int32
AF = mybir.ActivationFunctionType
ALU = mybir.AluOpType

NT = 2048
NG = 4
GS = 512
NCH = 16
EPS = 1e-6
NEG = -30000.0
SCALE = 192.0 ** -0.5
TWO_PI = 2.0 * math.pi
PAIRS = [[0, 1], [2, 3], [4, 5], [6, 7]]

C_PRE = 0
C_POST = 32
C_CW = 64
C_CB = 112
C_GN = 128
C_QAN = 144
C_KVAN = 150
C_INVF = 154
C_SGN = 155
C_ISB = 156
C_G128 = 160
C_CFIX = 164
NCF = 256
CB_TRI = 0
CB_ID = 128
CB_MASK = 256
CB_DQ = 384
CB_DK = 896
NCB = 1408
CB_ONES1 = 1408
CB_ONES128 = 1536
CB_ONES1024 = 1664
NCBF = 1792


class _Op:
    __slots__ = ("eng", "fn", "deps", "kind", "sig", "prev", "need", "sync", "out")

    def __init__(self, eng, fn, deps, kind):
        self.eng, self.fn, self.deps, self.kind = eng, fn, deps, kind
        self.sig = None
        self.prev = None
        self.need = False
        self.sync = []
        self.out = False


class Prog:
    NRING = 8

    def __init__(self, nc):
        self.nc = nc
        self.ops = []
        self.last_w = {}
        self.readers = {}

    def add(self, eng, fn, reads=(), writes=(), kind="c", out=False):
        idx = len(self.ops)
        deps = {}
        for k in reads:
            w = self.last_w.get(k)
            if w is not None:
                deps[w] = True
        for k in writes:
            w = self.last_w.get(k)
            if w is not None:
                deps.setdefault(w, False)
            for r in self.readers.get(k, ()):
                deps.setdefault(r, False)
        for k in reads:
            self.readers.setdefault(k, []).append(idx)
        for k in writes:
            self.last_w[k] = idx
            self.readers[k] = []
        deps.pop(idx, None)
        op = _Op(eng, fn, deps, kind)
        op.out = out
        self.ops.append(op)
        return idx

    def pe(self, fn, r=(), w=()):
        return self.add("pe", fn, r, w)

    def act(self, fn, r=(), w=()):
        return self.add("act", fn, r, w)

    def dve(self, fn, r=(), w=()):
        return self.add("dve", fn, r, w)

    def pool(self, fn, r=(), w=()):
        return self.add("pool", fn, r, w)

    def dma(self, q, fn, r=(), w=(), out=False):
        return self.add(q, fn, r, w, kind="d", out=out)

    def cc(self, fn, r=(), w=()):
        return self.add("pool", fn, r, w, kind="cc")

    @staticmethod
    def _sync_needed(p, c, raw):
        if p.kind != "c" or c.kind != "c":
            return True
        if p.eng != c.eng:
            return True
        if p.eng == "pe":
            return False
        return raw

    def emit(self):
        nc = self.nc
        ops = self.ops
        for op in ops:
            latest = {}
            for d, raw in op.deps.items():
                if self._sync_needed(ops[d], op, raw):
                    if ops[d].kind == "c":
                        pe_ = ops[d].eng
                        if latest.get(pe_, -1) < d:
                            latest[pe_] = d
                    else:
                        ops[d].need = True
                        op.sync.append(d)
            for d in latest.values():
                ops[d].need = True
                op.sync.append(d)
        EPOCH = 3000
        csem = {e: [nc.alloc_semaphore("c_%s0" % e)] for e in ("pe", "act", "dve", "pool")}
        ccnt = {e: 0 for e in csem}
        rings = {q: [nc.alloc_semaphore("d_%s%d" % (q, i)) for i in range(self.NRING)] for q in ("sp", "pool")}
        ruse = {q: [0] * self.NRING for q in rings}
        rpos = {q: 0 for q in rings}
        ccsem = nc.alloc_semaphore("ccsem")
        ccuse = 0
        for op in ops:
            if op.kind == "c":
                if op.need:
                    if ccnt[op.eng] >= EPOCH:
                        csem[op.eng].append(nc.alloc_semaphore("c_%s%d" % (op.eng, len(csem[op.eng]))))
                        ccnt[op.eng] = 0
                    ccnt[op.eng] += 1
                    op.sig = (csem[op.eng][-1], ccnt[op.eng], 1)
            elif op.kind == "d":
                q = op.eng
                i = rpos[q] % self.NRING
                rpos[q] += 1
                if ruse[q][i] > 0:
                    op.prev = (rings[q][i], 16 * ruse[q][i])
                ruse[q][i] += 1
                op.sig = (rings[q][i], 16 * ruse[q][i], 16)
            else:
                if ccuse > 0:
                    op.prev = (ccsem, ccuse)
                ccuse += 1
                op.sig = (ccsem, ccuse, 1)
        engmap = {"pe": "tensor", "act": "scalar", "dve": "vector", "pool": "gpsimd", "sp": "sync"}
        with nc.Block() as block:
            for ename, bname in engmap.items():
                mine = [op for op in ops if op.eng == ename]

                def body(e, mine=mine, ename=ename):
                    seen = {}

                    def wait(s, v):
                        if seen.get(s.num, 0) < v:
                            e.wait_ge(s, v)
                            seen[s.num] = v

                    for op in mine:
                        for d in op.sync:
                            s, v, _ = ops[d].sig
                            wait(s, v)
                        if op.prev is not None:
                            wait(*op.prev)
                        ins = op.fn(e)
                        if op.sig is not None:
                            ins.then_inc(op.sig[0], op.sig[2])
                    if ename == "sp":
                        for op in ops:
                            if op.out:
                                wait(op.sig[0], op.sig[1])

                getattr(block, bname)(body)


DEBUG = []


def build_program(layer_kinds, use_cc=True, debug=False):
    nc = bass.Bass("TRN2", target_bir_lowering=False)
    P = Prog(nc)
    NL = len(layer_kinds)

    def din(name, shape, dt=F32):
        return nc.dram_tensor(name, list(shape), dt, kind="ExternalInput").ap()

    xT_d = din("xT", [128, 8, NT])
    pos_d = din("pos", [128, NT], I32)
    cf_d = din("cf", [128, NCF])
    cb_d = din("cb", [128, NCB])
    mrow_d = din("mrow", [1, 2 * NT])
    W = []
    for li, kd in enumerate(layer_kinds):
        if kd == "e":
            W.append(dict(r=din("w%d_r" % li, [4, 128, 8, 1024]), c=din("w%d_c" % li, [8, 128, 8, 512]),
                          o=din("w%d_o" % li, [8, 128, 16, 128])))
        else:
            W.append(dict(i=din("w%d_i" % li, [128, 8, 768]), g=din("w%d_g" % li, [8, 128, 8, 128]),
                          q=din("w%d_q" % li, [8, 128, 3, 256]), kv=din("w%d_kv" % li, [8, 128, 2, 256]),
                          o=din("w%d_o" % li, [8, 128, 8, 128])))
    out_d = nc.dram_tensor("hT_out", [128, 8, NT], F32, kind="ExternalOutput").ap()
    mixd = nc.dram_tensor("mixd", [16, 128, NT], BF16).ap()
    cu_in = nc.dram_tensor("cu_in", [128, 16], BF16).ap()
    cu_out = nc.dram_tensor("cu_out", [256, 16], BF16).ap()
    ct_in = nc.dram_tensor("ct_in", [128, 128], F32).ap()
    ct_out = nc.dram_tensor("ct_out", [256, 128], F32).ap()
    cl_in = nc.dram_tensor("cl_in", [320, NT], BF16).ap()
    cl_out = nc.dram_tensor("cl_out", [640, NT], BF16).ap()

    hT = nc.alloc_sbuf_tensor("hT", [128, 8, NT], F32)
    uTr = nc.alloc_sbuf_tensor("uTr", [128, 8 * NT], BF16)
    mxr = nc.alloc_sbuf_tensor("mxr", [128, 8 * NT], BF16)
    ar = nc.alloc_sbuf_tensor("ar", [128, 37 * 512], BF16)
    uh_t = nc.alloc_sbuf_tensor("uh_t", [128, 2, 8, 2], BF16)
    cosT = nc.alloc_sbuf_tensor("cosT", [128, NT], BF16)
    sinT = nc.alloc_sbuf_tensor("sinT", [128, NT], BF16)
    NWB = 3
    wb = [nc.alloc_sbuf_tensor("wb%d" % i, [128, 4096], BF16) for i in range(NWB)]
    cf = nc.alloc_sbuf_tensor("cf_sb", [128, NCF], F32)
    cbf = nc.alloc_sbuf_tensor("cbf", [128, NCBF], BF16)
    sqb = [nc.alloc_sbuf_tensor("sqb%d" % i, [128, 512], BF16) for i in range(2)]
    rsb = nc.alloc_sbuf_tensor("rsb", [128, 512], F32)
    scr = nc.alloc_sbuf_tensor("scr", [128, 8], F32)
    onesf = nc.alloc_sbuf_tensor("onesf", [128, 128], F32)
    PS = [nc.alloc_psum_tensor("ps%d" % i, [128, 512], F32) for i in range(8)]

    def view(reg, off, shape, dt=BF16, p0=0, p1=128):
        n = 1
        for s in shape[1:]:
            n *= s
        if dt == BF16:
            a = reg[p0:p1, off // 2: off // 2 + n]
        else:
            a = reg[p0:p1, off // 2: off // 2 + 2 * n].bitcast(dt)
        if len(shape) == 3:
            a = a.rearrange("p (a b) -> p a b", a=shape[1])
        return a

    uT = view(uTr, 0, [128, 8, NT])
    tri = cbf[:, CB_TRI:CB_TRI + 128]
    ident = cbf[:, CB_ID:CB_ID + 128]
    ones1 = cbf[:, CB_ONES1:CB_ONES1 + 128]
    ones128 = cbf[:, CB_ONES128:CB_ONES128 + 128]
    ones1024 = cbf[:, CB_ONES1024:CB_ONES1024 + 128]

    def col(c, p0=0, p1=128):
        return cf[p0:p1, c:c + 1]

    K = 1024

    def mm(out, lhsT, rhs, start, stop):
        return lambda e: e.matmul(out, lhsT, rhs, start=start, stop=stop)

    def tt(out, a, b, op):
        return lambda e: e.tensor_tensor(out, a, b, op)

    def ts(out, a, s1, s2, op0, op1=None):
        if op1 is None:
            return lambda e: e.tensor_scalar(out, a, s1, None, op0)
        return lambda e: e.tensor_scalar(out, a, s1, s2, op0, op1)

    def stt(out, a, s, b, op0, op1):
        return lambda e: e.scalar_tensor_tensor(out, a, s, b, op0, op1)

    def actf(out, in_, func, bias=None, scale=None):
        kw = {}
        if bias is not None:
            kw["bias"] = bias
        if scale is not None:
            kw["scale"] = scale
        return lambda e: e.activation(out, in_, func, **kw)

    def cp(out, in_):
        return lambda e: e.tensor_copy(out, in_)

    def dmaf(out, in_):
        return lambda e: e.dma_start(out=out, in_=in_)

    bar_n = [0]

    def dbg(name, ap, keys):
        if not debug:
            return
        shp = list(ap.shape)
        d_ = nc.dram_tensor("dbg_" + name, shp, ap.dtype, kind="ExternalOutput").ap()
        DEBUG.append("dbg_" + name)
        P.dma("sp", dmaf(d_, ap), r=keys, out=True)

    def barrier(region_keys):
        bar_n[0] += 1
        P.dve(lambda e: e.memset(scr[:, 0:1], 0.0), r=(), w=list(region_keys) + [("scr",)])

    wplan = []
    for li_, kd_ in enumerate(layer_kinds):
        if kd_ == "e":
            for hp_ in range(4):
                wplan.append([W[li_]["r"][hp_][:, :, 0:512]])
                wplan.append([W[li_]["r"][hp_][:, :, 512:1024]])
            for i_ in range(8):
                wplan.append([W[li_]["c"][i_]])
        else:
            wplan.append([W[li_]["i"][:, :, 0:512]])
            wplan.append([W[li_]["i"][:, :, 512:768]])
            for h_ in range(8):
                wplan.append([W[li_]["q"][h_], W[li_]["kv"][h_], W[li_]["g"][h_]])
    wstate = {"issued": 0, "used": 0, "views": {}}

    def _issue(j):
        i = j % NWB
        key = ("wb", i)
        views = []
        off = 0
        for src in wplan[j]:
            a, b = src.shape[1], src.shape[2]
            v = wb[i][:, off:off + a * b].rearrange("p (a b) -> p a b", a=a)
            P.dma("pool", dmaf(v, src), r=(), w=[key])
            views.append(v)
            off += a * b
        assert off <= 4096
        wstate["views"][j] = (views, key)

    def wload(src_aps):
        j = wstate["used"]
        assert len(src_aps) == len(wplan[j])
        while wstate["issued"] < min(len(wplan), j + NWB):
            _issue(wstate["issued"])
            wstate["issued"] += 1
        wstate["used"] += 1
        return wstate["views"].pop(j)

    P.dma("sp", dmaf(cf[:], cf_d), w=[("cf",)])
    for q in range(4):
        P.dma("sp", dmaf(hT[:, 2 * q:2 * q + 2, :], xT_d[:, 2 * q:2 * q + 2, :]), w=[("hT", 2 * q), ("hT", 2 * q + 1)])
    posi = view(mxr, 0, [128, NT], I32)
    ang = view(mxr, 8192, [128, NT], F32)
    t_a = view(mxr, 16384, [128, NT], F32)
    t_n = view(mxr, 24576, [128, NT], I32)
    t_b = view(ar, 0, [128, NT], F32)
    t_c = view(ar, 8192, [128, NT], F32)
    P.dma("sp", dmaf(posi, pos_d), w=[("posi",)])
    cbst = view(ar, 16384, [128, NCB], F32)
    P.dma("sp", dmaf(cbst, cb_d), w=[("cbst",)])
    P.dve(cp(cbf[:, 0:NCB], cbst), r=[("cbst",)], w=[("cbf",)])
    P.pool(lambda e: e.memset(onesf[:], 1.0), w=[("cbf1",)])
    P.pool(lambda e: e.memset(ones1, 1.0), w=[("cbf1",)])
    P.pool(lambda e: e.memset(ones128, 1.0 / 128), w=[("cbf1",)])
    P.pool(lambda e: e.memset(ones1024, 1.0 / 1024), w=[("cbf1",)])
    P.dve(lambda e: e.memset(scr[:, 1:2], 0.0), r=[("cbf1",)], w=[("cbf",)])
    P.dve(cp(ang, posi), r=[("posi",)], w=[("ang",)])
    P.dve(ts(ang, ang, col(C_INVF), None, ALU.mult), r=[("ang",), ("cf",)], w=[("ang",)])
    for which, shift, dst in (("s", 0.0, sinT), ("c", 0.5 * math.pi, cosT)):
        P.dve(ts(t_a, ang, shift, 1.0 / TWO_PI, ALU.add, ALU.mult), r=[("ang",)], w=[("t_a",)])
        P.dve(cp(t_n, t_a), r=[("t_a",)], w=[("t_n",)])
        P.dve(cp(t_b, t_n), r=[("t_n",)], w=[("t_b",)])
        P.dve(ts(t_a, ang, shift, None, ALU.add), r=[("ang",), ("t_n",)], w=[("t_a",)])
        P.dve(stt(t_b, t_b, -TWO_PI, t_a, ALU.mult, ALU.add), r=[("t_b",), ("t_a",)], w=[("t_b",)])
        P.dve(ts(t_c, t_b, math.pi, -TWO_PI, ALU.is_gt, ALU.mult), r=[("t_b",)], w=[("t_c",)])
        P.dve(tt(t_b, t_b, t_c, ALU.add), r=[("t_b",), ("t_c",)], w=[("t_b",)])
        P.dve(ts(t_c, t_b, -math.pi, TWO_PI, ALU.is_lt, ALU.mult), r=[("t_b",)], w=[("t_c",)])
        P.dve(tt(t_b, t_b, t_c, ALU.add), r=[("t_b",), ("t_c",)], w=[("t_b",)])
        P.dve(ts(t_b, t_b, math.pi, -math.pi, ALU.min, ALU.max), r=[("t_b",)], w=[("t_b",)])
        if which == "s":
            P.act(actf(t_c, t_b, AF.Sin), r=[("t_b",)], w=[("t_c",)])
            P.dve(ts(dst[:], t_c, col(C_SGN), None, ALU.mult), r=[("t_c",), ("cf",)], w=[("rope",)])
        else:
            P.act(actf(dst[:], t_b, AF.Sin), r=[("t_b",)], w=[("rope",)])

    REG_U = [("Ru",)]
    REG_M = [("Rm",)]
    REG_A = [("Ra",)]
    barrier(REG_U + REG_M + REG_A + [("t_a",), ("t_b",), ("t_c",), ("t_n",), ("ang",), ("posi",), ("cbst",)])

    def rstd_from(psn, scale, rkeys):
        P.act(actf(rsb[:], psn, AF.Sqrt, bias=col(C_EPS_), scale=scale), r=rkeys + [("cf",)], w=[("rsb",)])
        P.dve(lambda e: e.reciprocal(rsb[:], rsb[:]), r=[("rsb",)], w=[("rsb",)])

    def prenorm(l):
        for g in range(NG):
            tok = slice(g * GS, (g + 1) * GS)
            for kc in range(8):
                s = sqb[kc % 2]
                P.pool(tt(s[:], hT[:, kc, tok], hT[:, kc, tok], ALU.mult), r=[("hT", kc)], w=[("sqb", kc % 2)])
                P.pe(mm(PS[6][:], ones1024, s[:], kc == 0, kc == 7), r=[("sqb", kc % 2), ("cbf",)], w=[("ps", 6)])
            rstd_from(PS[6][:], 1.0, [("ps", 6)])
            for kc in range(8):
                P.dve(stt(uT[:, kc, tok], hT[:, kc, tok], col(C_PRE + l * 8 + kc), rsb[:], ALU.mult, ALU.mult),
                      r=[("hT", kc), ("rsb",), ("cf",)] + REG_U, w=[("uT", kc, g)])

    def load_wo(nf, wo_d):
        barrier(REG_M)
        wo = view(mxr, 0, [128, nf, 1024])
        for cc_ in range(8):
            P.dma("pool", dmaf(wo[:, :, cc_ * 128:(cc_ + 1) * 128], wo_d[cc_]), r=REG_M, w=[("wo", cc_)])

    def outproj(l, nf, wo_d, post_col, preloaded=False):
        barrier(REG_U)
        if not preloaded:
            load_wo(nf, wo_d)
        wo = view(mxr, 0, [128, nf, 1024])
        yT = view(uTr, 0, [128, 8, 512], F32)
        nmb = 2 if nf == 8 else 1
        mixgs = [view(uTr, 16384 + 8192 * i, [128, nf, 512]) for i in range(nmb)]
        for g in range(NG):
            tok = slice(g * GS, (g + 1) * GS)
            mixg = mixgs[g % nmb]
            mgk = ("mixg", g % nmb)
            P.dma("sp", dmaf(mixg, mixd[0:nf, :, tok].rearrange("f p t -> p f t")),
                  r=REG_U + [("mixd", f) for f in range(nf)], w=[mgk])
            for cc_ in range(8):
                ps = PS[cc_ % 2]
                pk = ("ps", cc_ % 2)
                for f in range(nf):
                    P.pe(mm(ps[:], wo[:, f, cc_ * 128:(cc_ + 1) * 128], mixg[:, f, :], f == 0, f == nf - 1),
                         r=[("wo", cc_), mgk], w=[pk])
                P.act(actf(yT[:, cc_, :], ps[:], AF.Copy), r=[pk] + REG_U, w=[("yT", cc_)])
                s = sqb[cc_ % 2]
                P.act(actf(s[:], ps[:], AF.Square), r=[pk], w=[("sqb", cc_ % 2)])
                P.pe(mm(PS[6][:], ones1024, s[:], cc_ == 0, cc_ == 7), r=[("sqb", cc_ % 2), ("cbf",)], w=[("ps", 6)])
            rstd_from(PS[6][:], 1.0, [("ps", 6)])
            for cc_ in range(8):
                P.dve(stt(yT[:, cc_, :], yT[:, cc_, :], col(post_col + cc_), rsb[:], ALU.mult, ALU.mult),
                      r=[("yT", cc_), ("rsb",), ("cf",)], w=[("yT", cc_)])
                P.pool(tt(hT[:, cc_, tok], hT[:, cc_, tok], yT[:, cc_, :], ALU.add), r=[("yT", cc_), ("hT", cc_)],
                       w=[("hT", cc_)])
        barrier(REG_U)
        barrier(REG_M)

    def exchange(src_list, cin, cout, dst_list, keys_r, keys_w, nrows):
        for rows, sap in src_list:
            P.dma("sp", dmaf(cin[rows], sap), r=keys_r, w=[("cin", id(cin))])
        if use_cc:
            P.cc(lambda e: e.collective_compute("AllGather", ALU.bypass, replica_groups=PAIRS, ins=[cin], outs=[cout]),
                 r=[("cin", id(cin))], w=[("cout", id(cout))])
        else:
            P.dma("sp", dmaf(cout[0:nrows], cin), r=[("cin", id(cin))], w=[("cout", id(cout))])
        for sap, rows in dst_list:
            P.dma("sp", dmaf(sap, cout[rows]), r=[("cout", id(cout))] + REG_A + REG_M, w=keys_w)

    def even_layer(l, e_i, Wl):
        prenorm(l)
        uh = uh_t[:, 0, :, :]
        uh_own = uh_t[:, 1, :, :]
        for kc in range(8):
            P.dve(cp(uh_own[:, kc, :], uT[:, kc, NT - 2:NT]), r=[("uT", kc, 3)] + REG_A, w=[("uh_own",)])
        exchange([(slice(0, 128), uh_own.rearrange("p a b -> p (a b)"))], cu_in, cu_out,
                 [(uh.rearrange("p a b -> p (a b)"), slice(0, 128))], [("uh_own",)], [("uh",)], 128)

        qT = view(mxr, 0, [128, NT])
        kT = view(mxr, 4096, [128, NT])
        Kt = view(mxr, 8192, [128, NCH, 128])
        V = view(mxr, 12288, [128, NCH, 256])
        sg = view(mxr, 20480, [128, 2, NT])
        Tb = view(mxr, 28672, [128, NCH, 128])
        Tf = view(ar, 0, [128, 17, 128], F32)
        Tin = view(ar, 8704, [128, 128], F32)
        t1 = view(ar, 9216, [128, 512], F32)
        t2 = view(ar, 11264, [128, 512], F32)
        PT = [view(ar, 13312 + 256 * i, [128, 128]) for i in range(4)]
        stage = [view(ar, 14336 + 4096 * i, [128, NT]) for i in range(2)]
        zb = view(ar, 22528, [128, 2 + NT], F32)
        cxs = view(ar, 30752, [128, 512], F32)
        acc = view(ar, 32800, [128, 512], F32)
        sgc = view(ar, 34848, [128, 512])
        zh = view(ar, 35872, [128, 2], F32)
        mask01 = cbf[:, CB_MASK:CB_MASK + 128]
        gam = [1.0 - 2.0 ** (-5.0 - h) for h in range(8)]

        def rope_evac(psa, psb, g, dst, dec, pk_a, pk_b, dkey):
            tok = slice(g * GS, (g + 1) * GS)
            P.dve(tt(t1, psa, cosT[:, tok], ALU.mult), r=[pk_a, ("rope",)] + REG_A, w=[("t1",)])
            P.dve(tt(t2, psb, sinT[:, tok], ALU.mult), r=[pk_b, ("rope",)] + REG_A, w=[("t2",)])
            P.pool(tt(t1, t1, t2, ALU.add), r=[("t1",), ("t2",)], w=[("t1",)])
            P.dve(tt(dst[:, tok].rearrange("p (c i) -> p c i", c=4), t1.rearrange("p (c i) -> p c i", c=4),
                     dec.unsqueeze(1).to_broadcast([128, 4, 128]), ALU.mult),
                  r=[("t1",), ("cbf",)] + REG_M, w=[dkey])

        for hp in range(4):
            (wA,), wk_key = wload([Wl["r"][hp][:, :, 0:512]])
            for g in range(NG):
                tok = slice(g * GS, (g + 1) * GS)
                pb = 4 * (g % 2)
                for j, pi in ((0, pb), (1, pb + 1)):
                    for kc in range(8):
                        P.pe(mm(PS[pi][:], wA[:, kc, j * 128:(j + 1) * 128], uT[:, kc, tok], kc == 0, kc == 7),
                             r=[wk_key, ("uT", kc, g)], w=[("ps", pi)])
                rope_evac(PS[pb][:], PS[pb + 1][:], g, kT, cbf[:, CB_DK + hp * 128:CB_DK + (hp + 1) * 128],
                          ("ps", pb), ("ps", pb + 1), ("kT", g))
                for c in range(4 * g, 4 * g + 4):
                    pT = PS[7][:, 0:64].bitcast(BF16)
                    P.pe(lambda e, c=c, pT=pT: e.transpose(pT, kT[:, c * 128:(c + 1) * 128], ident),
                         r=[("kT", g), ("cbf",)], w=[("ps", 7)])
                    for hh in range(2):
                        gv = gam[2 * hp + hh] ** 128
                        P.act(actf(Kt[:, c, hh * 64:(hh + 1) * 64], pT[:, hh * 64:(hh + 1) * 64], AF.Copy, scale=gv),
                              r=[("ps", 7)] + REG_M, w=[("Kt", c)])
                    pv = PS[2 + (c % 2)]
                    for kc in range(8):
                        P.pe(mm(pv[:, 0:256], uT[:, kc, c * 128:(c + 1) * 128], wA[:, kc, 256:512], kc == 0, kc == 7),
                             r=[wk_key, ("uT", kc, g)], w=[("ps", 2 + (c % 2))])
                    P.act(actf(V[:, c, :], pv[:, 0:256], AF.Copy), r=[("ps", 2 + (c % 2))] + REG_M, w=[("V", c)])
            P.dve(lambda e: e.memset(Tf[:, 0, :], 0.0), r=REG_A, w=[("Tf", 0)])
            for c in range(NCH):
                for hh in range(2):
                    rows = slice(64 * hh, 64 * hh + 64)
                    pk = ("ps", 4 + hh)
                    P.pe(mm(PS[4 + hh][:, 0:128], Kt[:, c, :], V[:, c, hh * 128:(hh + 1) * 128], True, True),
                         r=[("Kt", c), ("V", c)], w=[pk])
                    P.dve(stt(Tf[rows, c + 1, :], Tf[rows, c, :], col(C_G128 + hp, 64 * hh, 64 * hh + 64),
                              PS[4 + hh][rows, 0:128], ALU.mult, ALU.add),
                          r=[pk, ("Tf", c), ("cf",)], w=[("Tf", c + 1)])
            exchange([(slice(0, 128), Tf[:, 16, :])], ct_in, ct_out, [(Tin, slice(0, 128))], [("Tf", 16)], [("Tin",)], 128)

            (wB,), wq_key = wload([Wl["r"][hp][:, :, 512:1024]])
            for g in range(NG):
                tok = slice(g * GS, (g + 1) * GS)
                pb = 4 * (g % 2)
                for j, pi in ((0, pb), (1, pb + 1)):
                    for kc in range(8):
                        P.pe(mm(PS[pi][:], wB[:, kc, j * 128:(j + 1) * 128], uT[:, kc, tok], kc == 0, kc == 7),
                             r=[wq_key, ("uT", kc, g)], w=[("ps", pi)])
                rope_evac(PS[pb][:], PS[pb + 1][:], g, qT, cbf[:, CB_DQ + hp * 128:CB_DQ + (hp + 1) * 128],
                          ("ps", pb), ("ps", pb + 1), ("qT", g))
                for hh in range(2):
                    gi = pb + 2 + hh
                    for kc in range(8):
                        P.pe(mm(PS[gi][:], wB[:, kc, 256 + hh * 128:256 + (hh + 1) * 128], uT[:, kc, tok],
                                kc == 0, kc == 7), r=[wq_key, ("uT", kc, g)], w=[("ps", gi)])
                    P.act(actf(sg[:, hh, tok], PS[gi][:], AF.Silu), r=[("ps", gi)] + REG_M, w=[("sg", hh, g)])
            for n in range(NCH):
                P.dve(stt(Tb[:, n, :], Tin, col(C_CFIX + hp * 16 + n), Tf[:, n, :], ALU.mult, ALU.add),
                      r=[("Tin",), ("Tf", n), ("cf",)] + REG_M, w=[("Tb", n)])
            def emit_ST(c):
                g = c // 4
                cs = slice(c * 128, (c + 1) * 128)
                for hh in range(2):
                    rows = slice(64 * hh, 64 * hh + 64)
                    si = hh + 4 * (c % 2)
                    P.pe(mm(PS[si][:, 0:128], kT[rows, cs], qT[rows, cs], True, True), r=[("kT", g), ("qT", g)],
                         w=[("ps", si)])

            emit_ST(0)
            for c in range(NCH):
                g, cl = c // 4, c % 4
                cs = slice(c * 128, (c + 1) * 128)
                if c + 1 < NCH:
                    emit_ST(c + 1)
                for hh in range(2):
                    rows = slice(64 * hh, 64 * hh + 64)
                    si = hh + 4 * (c % 2)
                    pS = PS[si]
                    pR = PS[2 + hh]
                    pt = PT[2 * hh + (c % 2)]
                    ptk = ("PT", 2 * hh + (c % 2))
                    P.dve(tt(pt, pS[:, 0:128], mask01, ALU.mult), r=[("ps", si), ("cbf",)] + REG_A, w=[ptk])
                    P.pe(mm(pR[:, cl * 128:(cl + 1) * 128], V[:, c, hh * 128:(hh + 1) * 128], pt, True, False),
                         r=[("V", c), ptk], w=[("ps", 2 + hh)])
                    P.pe(mm(pR[:, cl * 128:(cl + 1) * 128], Tb[rows, c, :], qT[rows, cs], False, True),
                         r=[("Tb", c), ("qT", g)], w=[("ps", 2 + hh)])
                if cl == 3:
                    tok = slice(g * GS, (g + 1) * GS)
                    for hh in range(2):
                        pR = PS[2 + hh]
                        s = sqb[hh]
                        P.act(actf(s[:], pR[:], AF.Square), r=[("ps", 2 + hh)], w=[("sqb", hh)])
                        P.pe(mm(PS[6][:], ones128, s[:], True, True), r=[("sqb", hh), ("cbf",)], w=[("ps", 6)])
                        rstd_from(PS[6][:], 1.0, [("ps", 6)])
                        P.dve(stt(t1, pR[:], col(C_GN + e_i * 8 + 2 * hp + hh), rsb[:], ALU.mult, ALU.mult),
                              r=[("ps", 2 + hh), ("rsb",), ("cf",)] + REG_A, w=[("t1",)])
                        P.pool(tt(stage[hh][:, tok], t1, sg[:, hh, tok], ALU.mult), r=[("t1",), ("sg", hh, g)],
                               w=[("stage", hh)])
            for hh in range(2):
                P.dma("sp", dmaf(mixd[2 * hp + hh], stage[hh]), r=[("stage", hh)], w=[("mixd", 2 * hp + hh)])

        load_wo(16, Wl["o"])
        for i in range(8):
            (wC,), wc_key = wload([Wl["c"][i]])
            pH = PS[4]
            for j, cb_ in ((0, 128), (1, 256)):
                for kc in range(8):
                    P.pe(mm(pH[:, 2 * j:2 * j + 2], wC[:, kc, cb_:cb_ + 128], uh[:, kc, :], kc == 0, kc == 7),
                         r=[wc_key, ("uh",)], w=[("ps", 4)])
            P.act(actf(zh, pH[:, 2:4], AF.Copy), r=[("ps", 4)] + REG_A, w=[("zh",)])
            P.dve(stt(zb[:, 0:2], pH[:, 0:2], col(C_ISB), zh, ALU.mult, ALU.mult), r=[("ps", 4), ("zh",), ("cf",)],
                  w=[("zb", -1)])
            st = stage[i % 2]
            for g in range(NG):
                tok = slice(g * GS, (g + 1) * GS)
                pb = 4 * (g % 2)
                for j in (2, 1, 3, 0):
                    for kc in range(8):
                        P.pe(mm(PS[pb + j][:], wC[:, kc, j * 128:(j + 1) * 128], uT[:, kc, tok], kc == 0, kc == 7),
                             r=[wc_key, ("uT", kc, g)], w=[("ps", pb + j)])
                P.act(actf(cxs, PS[pb + 2][:], AF.Copy), r=[("ps", pb + 2)] + REG_A, w=[("cxs",)])
                P.act(actf(sgc, PS[pb + 3][:], AF.Silu), r=[("ps", pb + 3)] + REG_A, w=[("sgc",)])
                P.dve(tt(zb[:, 2 + g * GS:2 + (g + 1) * GS], PS[pb + 1][:], cxs, ALU.mult), r=[("ps", pb + 1), ("cxs",)],
                      w=[("zb", g)])
                zr = [("zb", g), ("zb", g - 1), ("cf",)]
                cw = C_CW + e_i * 24 + i
                P.dve(ts(acc, zb[:, g * GS:g * GS + GS], col(cw), None, ALU.mult), r=zr, w=[("acc",)])
                P.dve(stt(acc, zb[:, g * GS + 1:g * GS + 1 + GS], col(cw + 8), acc, ALU.mult, ALU.add),
                      r=zr + [("acc",)], w=[("acc",)])
                P.dve(stt(acc, zb[:, g * GS + 2:g * GS + 2 + GS], col(cw + 16), acc, ALU.mult, ALU.add),
                      r=zr + [("acc",)], w=[("acc",)])
                P.dve(stt(acc, acc, col(C_CB + e_i * 8 + i), PS[pb][:], ALU.add, ALU.mult),
                      r=[("acc",), ("ps", pb), ("cf",)], w=[("acc",)])
                P.pool(tt(st[:, tok], acc, sgc, ALU.mult), r=[("acc",), ("sgc",)], w=[("stage", i % 2)])
            P.dma("sp", dmaf(mixd[8 + i], st), r=[("stage", i % 2)], w=[("mixd", 8 + i)])

        outproj(l, 16, Wl["o"], C_POST + l * 8, preloaded=True)
        barrier(REG_A)

    def odd_layer(l, o_i, Wl):
        prenorm(l)
        latT = view(mxr, 0, [128, 2, 2 * NT])
        krT = view(mxr, 16384, [128, 2 * NT])
        knT = view(mxr, 24576, [128, 2 * NT])
        cqT = view(ar, 0, [128, 3, NT])
        Vv = view(ar, 12288, [128, 32, 128])
        qnT = view(ar, 20480, [128, NT])
        qrT = view(ar, 24576, [128, NT])
        PTs = [view(ar, 28672 + 1024 * i, [128, 512]) for i in range(2)] + [view(ar, 36864, [128, 512])]
        sgh = view(ar, 30720, [128, NT])
        t1 = view(ar, 34816, [128, 512], F32)
        t2 = view(ar, 36864, [128, 256], F32)
        (wI,), wi_key = wload([Wl["i"][:, :, 0:512]])
        P.pool(lambda e: e.memset(krT[64:96, :], 0.0), r=REG_M, w=[("krT", "m")])
        P.dma("pool", dmaf(krT[64:65, :], mrow_d), r=REG_M, w=[("krT", "m")])
        P.pool(lambda e: e.memset(qrT[64:96, :], 0.0), r=REG_A, w=[("qrT", "m")])
        P.pool(lambda e: e.memset(qrT[64:65, :], NEG), r=REG_A, w=[("qrT", "m")])
        for g in range(NG):
            tok = slice(g * GS, (g + 1) * GS)
            otok = slice(NT + g * GS, NT + (g + 1) * GS)
            for j in range(3):
                for kc in range(8):
                    P.pe(mm(PS[j][:], wI[:, kc, j * 128:(j + 1) * 128], uT[:, kc, tok], kc == 0, kc == 7),
                         r=[wi_key, ("uT", kc, g)], w=[("ps", j)])
                s = sqb[j % 2]
                P.act(actf(s[:], PS[j][:], AF.Square), r=[("ps", j)], w=[("sqb", j % 2)])
                P.pe(mm(PS[6][:], ones128, s[:], j == 0, j == 2), r=[("sqb", j % 2), ("cbf",)], w=[("ps", 6)])
            rstd_from(PS[6][:], 1.0 / 3.0, [("ps", 6)])
            for j in range(3):
                P.dve(stt(cqT[:, j, tok], PS[j][:], col(C_QAN + o_i * 3 + j), rsb[:], ALU.mult, ALU.mult),
                      r=[("ps", j), ("rsb",), ("cf",)] + REG_A, w=[("cqT", g)])
            for j, pi in ((0, 3), (1, 4)):
                for kc in range(8):
                    P.pe(mm(PS[pi][0:64, :], wI[:, kc, 384 + 64 * j:448 + 64 * j], uT[:, kc, tok], kc == 0, kc == 7),
                         r=[wi_key, ("uT", kc, g)], w=[("ps", pi)])
            P.dve(tt(t1[0:64, :], PS[3][0:64, :], cosT[0:64, tok], ALU.mult), r=[("ps", 3), ("rope",)] + REG_A, w=[("t1",)])
            P.dve(tt(rsb[0:64, :], PS[4][0:64, :], sinT[0:64, tok], ALU.mult), r=[("ps", 4), ("rope",), ("rsb",)],
                  w=[("rsb",)])
            P.pool(tt(krT[0:64, otok], t1[0:64, :], rsb[0:64, :], ALU.add), r=[("t1",), ("rsb",)] + REG_M,
                   w=[("krT", 4 + g)])
        (wI2,), wi2_key = wload([Wl["i"][:, :, 512:768]])
        for g in range(NG):
            tok = slice(g * GS, (g + 1) * GS)
            otok = slice(NT + g * GS, NT + (g + 1) * GS)
            for j in range(2):
                for kc in range(8):
                    P.pe(mm(PS[j][:], wI2[:, kc, j * 128:(j + 1) * 128], uT[:, kc, tok], kc == 0, kc == 7),
                         r=[wi2_key, ("uT", kc, g)], w=[("ps", j)])
                s = sqb[j % 2]
                P.act(actf(s[:], PS[j][:], AF.Square), r=[("ps", j)], w=[("sqb", j % 2)])
                P.pe(mm(PS[6][:], ones128, s[:], j == 0, j == 1), r=[("sqb", j % 2), ("cbf",)], w=[("ps", 6)])
            rstd_from(PS[6][:], 0.5, [("ps", 6)])
            for j in range(2):
                P.dve(stt(latT[:, j, otok], PS[j][:], col(C_KVAN + o_i * 2 + j), rsb[:], ALU.mult, ALU.mult),
                      r=[("ps", j), ("rsb",), ("cf",)] + REG_M, w=[("latT", 4 + g)])
        own = slice(NT, 2 * NT)
        prv = slice(0, NT)
        lk = [("latT", 4 + g) for g in range(NG)] + [("krT", 4 + g) for g in range(NG)]
        pk_ = [("latT", g) for g in range(NG)] + [("krT", g) for g in range(NG)]
        exchange([(slice(0, 128), latT[:, 0, own]), (slice(128, 256), latT[:, 1, own]), (slice(256, 320), krT[0:64, own])],
                 cl_in, cl_out,
                 [(latT[:, 0, prv], slice(0, 128)), (latT[:, 1, prv], slice(128, 256)), (krT[0:64, prv], slice(256, 320))],
                 lk, pk_, 320)
        allk = [("latT", g) for g in range(8)]
        allkr = [("krT", g) for g in range(8)] + [("krT", "m")]

        for h in range(8):
            (wq, wkv, wg), wh_key = wload([Wl["q"][h], Wl["kv"][h], Wl["g"][h]])
            for kg in range(8):
                ks = slice(kg * 512, (kg + 1) * 512)
                pi = kg % 2
                for kc in range(2):
                    P.pe(mm(PS[pi][:], wkv[:, kc, 0:128], latT[:, kc, ks], kc == 0, kc == 1),
                         r=[wh_key, ("latT", kg)], w=[("ps", pi)])
                if kg % 2 == 0:
                    P.act(actf(knT[:, ks], PS[pi][:], AF.Copy), r=[("ps", pi)] + REG_M, w=[("knT", kg)])
                else:
                    P.dve(cp(knT[:, ks], PS[pi][:]), r=[("ps", pi)] + REG_M, w=[("knT", kg)])
            for kg in range(8):
                pi = 2 + kg % 2
                for j in range(4):
                    kb = 4 * kg + j
                    for kc in range(2):
                        P.pe(mm(PS[pi][:, j * 128:(j + 1) * 128], latT[:, kc, kb * 128:(kb + 1) * 128], wkv[:, kc, 128:256],
                                kc == 0, kc == 1), r=[wh_key, ("latT", kg)], w=[("ps", pi)])
                dstv = Vv[:, 4 * kg:4 * kg + 4, :].rearrange("p a b -> p (a b)")
                if kg % 2 == 0:
                    P.dve(cp(dstv, PS[pi][:]), r=[("ps", pi)] + REG_A, w=[("Vv", kg)])
                else:
                    P.act(actf(dstv, PS[pi][:], AF.Copy), r=[("ps", pi)] + REG_A, w=[("Vv", kg)])
            for g in range(NG):
                tok = slice(g * GS, (g + 1) * GS)
                for kc in range(3):
                    P.pe(mm(PS[0][:], wq[:, kc, 0:128], cqT[:, kc, tok], kc == 0, kc == 2),
                         r=[wh_key, ("cqT", g)], w=[("ps", 0)])
                P.act(actf(qnT[:, tok], PS[0][:], AF.Copy, scale=SCALE), r=[("ps", 0)] + REG_A, w=[("qnT", g)])
                for j, pi in ((0, 1), (1, 2)):
                    for kc in range(3):
                        P.pe(mm(PS[pi][0:64, :], wq[:, kc, 128 + 64 * j:192 + 64 * j], cqT[:, kc, tok], kc == 0, kc == 2),
                             r=[wh_key, ("cqT", g)], w=[("ps", pi)])
                P.dve(stt(t1[0:64, :], PS[1][0:64, :], SCALE, cosT[0:64, tok], ALU.mult, ALU.mult),
                      r=[("ps", 1), ("rope",)] + REG_A, w=[("t1",)])
                P.dve(stt(rsb[0:64, :], PS[2][0:64, :], SCALE, sinT[0:64, tok], ALU.mult, ALU.mult),
                      r=[("ps", 2), ("rope",), ("rsb",)], w=[("rsb",)])
                P.pool(tt(qrT[0:64, tok], t1[0:64, :], rsb[0:64, :], ALU.add), r=[("t1",), ("rsb",)] + REG_A,
                       w=[("qrT", g)])
                for kc in range(8):
                    P.pe(mm(PS[3][:], wg[:, kc, :], uT[:, kc, tok], kc == 0, kc == 7), r=[wh_key, ("uT", kc, g)],
                         w=[("ps", 3)])
                P.act(actf(sgh[:, tok], PS[3][:], AF.Silu), r=[("ps", 3)] + REG_A, w=[("sgh", g)])
            if h == 0:
                dbg("cqT", cqT.rearrange("p a b -> p (a b)"), [("cqT", g) for g in range(NG)])
                dbg("latT", latT.rearrange("p a b -> p (a b)"), allk)
                dbg("krT", krT[0:96, :], allkr)
                dbg("knT", knT, [("knT", kg) for kg in range(8)])
                dbg("Vv", Vv.rearrange("p a b -> p (a b)"), [("Vv", kg) for kg in range(8)])
                dbg("qnT", qnT, [("qnT", g) for g in range(NG)])
                dbg("qrT", qrT[0:96, :], [("qrT", g) for g in range(NG)] + [("qrT", "m")])
                dbg("gate", sgh, [("sgh", g) for g in range(NG)])
            knk = [("knT", kg) for kg in range(8)]
            vk = [("Vv", kg) for kg in range(8)]
            for qg in range(NG):
                qtok0 = qg * GS
                blocks = [(kb, 0) for kb in range(16 + 4 * qg)] + [(16 + 4 * qg + t, t) for t in range(4)]
                nb = len(blocks)

                def emit_S(bi):
                    kb, t = blocks[bi]
                    diag = kb >= 16 + 4 * qg
                    q0 = qtok0 + 128 * t
                    n = GS - 128 * t
                    pS = PS[3 + bi % 3]
                    pk = ("ps", 3 + bi % 3)
                    ks = slice(kb * 128, (kb + 1) * 128)
                    P.pe(mm(pS[:, 0:n], knT[:, ks], qnT[:, q0:q0 + n], True, False), r=knk + [("qnT", qg)], w=[pk])
                    P.pe(mm(pS[:, 0:n], krT[0:96, ks], qrT[0:96, q0:q0 + n], False, not diag),
                         r=allkr + [("qrT", qg), ("qrT", "m")], w=[pk])
                    if diag:
                        P.pe(mm(pS[:, 0:128], ident, tri, False, True), r=[("cbf",)], w=[pk])

                def emit_PV(bi):
                    kb, t = blocks[bi]
                    n = GS - 128 * t
                    pS = PS[3 + bi % 3]
                    pk = ("ps", 3 + bi % 3)
                    pt = PTs[bi % 3]
                    P.act(actf(pt[:, 0:n], pS[:, 0:n], AF.Exp), r=[pk] + REG_A, w=[("PTs", bi % 3)])
                    P.pe(mm(PS[6][:, 128 * t:GS], Vv[:, kb, :], pt[:, 0:n], bi == 0, bi == nb - 1),
                         r=vk + [("PTs", bi % 3)], w=[("ps", 6)])
                    accL, ak = (rsb[:], ("rsb",)) if bi % 2 == 0 else (t1, ("t1",))
                    if bi < 2:
                        P.dve(cp(accL, pt[:, 0:n]), r=[("PTs", bi % 3), ak] + REG_A, w=[ak])
                    else:
                        P.dve(tt(accL[:, 128 * t:GS], accL[:, 128 * t:GS], pt[:, 0:n], ALU.add),
                              r=[("PTs", bi % 3), ak], w=[ak])

                emit_S(0)
                emit_S(1)
                for bi in range(nb):
                    if bi + 2 < nb:
                        emit_S(bi + 2)
                    emit_PV(bi)
                tok = slice(qtok0, qtok0 + GS)
                P.dve(tt(rsb[:], rsb[:], t1, ALU.add), r=[("rsb",), ("t1",)], w=[("rsb",)])
                P.pe(mm(PS[7][:], onesf[:], rsb[:], True, True), r=[("cbf",), ("rsb",)], w=[("ps", 7)])
                P.dve(lambda e: e.reciprocal(t1, PS[7][:]), r=[("ps", 7)] + REG_A, w=[("t1",)])
                P.dve(tt(t1, PS[6][:], t1, ALU.mult), r=[("ps", 6), ("t1",)], w=[("t1",)])
                P.pool(tt(sgh[:, tok], t1, sgh[:, tok], ALU.mult), r=[("t1",), ("sgh", qg)], w=[("sgh", qg)])
            if h == 0:
                dbg("og", sgh, [("sgh", g) for g in range(NG)])
            P.dma("sp", dmaf(mixd[h], sgh), r=[("sgh", g) for g in range(NG)], w=[("mixd", h)])

        outproj(l, 8, Wl["o"], C_POST + l * 8)
        barrier(REG_A)

    e_i = 0
    o_i = 0
    for l, kd in enumerate(layer_kinds):
        if kd == "e":
            even_layer(l, e_i, W[l])
            e_i += 1
        else:
            odd_layer(l, o_i, W[l])
            o_i += 1

    for q in range(4):
        P.dma("sp", dmaf(out_d[:, 2 * q:2 * q + 2, :], hT[:, 2 * q:2 * q + 2, :]), r=[("hT", 2 * q), ("hT", 2 * q + 1)],
              out=True)
    P.emit()
    return nc


C_EPS_ = 157


def _feat_major(a):
    t = a.shape[0]
    return np.ascontiguousarray(a.T.reshape(8, 128, t).transpose(1, 0, 2))


def _from_feat_major(a):
    t = a.shape[2]
    return np.ascontiguousarray(a.transpose(1, 0, 2).reshape(1024, t).T)


def _kmajor(w):
    k, c = w.shape
    return np.ascontiguousarray(w.reshape(k // 128, 128, c).transpose(1, 0, 2))


def _swap_cols(n_heads, d):
    idx = np.arange(n_heads * d).reshape(n_heads, 2, d // 2)
    return idx[:, ::-1, :].reshape(-1)


def _const_tables(core, layers, inp):
    cf = np.zeros((128, NCF), np.float32)
    cb = np.zeros((128, NCB), np.float32)
    p = np.arange(128)
    e_i = o_i = 0
    for li, l in enumerate(layers):
        cf[:, C_PRE + li * 8:C_PRE + li * 8 + 8] = inp["pre_norm"][l].reshape(8, 128).T
        cf[:, C_POST + li * 8:C_POST + li * 8 + 8] = inp["post_norm"][l].reshape(8, 128).T
        if l % 2 == 0:
            e = l // 2
            for tap in range(3):
                cf[:, C_CW + e_i * 24 + tap * 8:C_CW + e_i * 24 + tap * 8 + 8] = inp["even_conv_w"][e][tap].reshape(8, 128).T
            cf[:, C_CB + e_i * 8:C_CB + e_i * 8 + 8] = inp["even_conv_b"][e].reshape(8, 128).T
            cf[:, C_GN + e_i * 8:C_GN + e_i * 8 + 8] = inp["ret_gn"][e].reshape(8, 128).T
            e_i += 1
        else:
            o = l // 2
            cf[:, C_QAN + o_i * 3:C_QAN + o_i * 3 + 3] = inp["q_a_norm"][o].reshape(3, 128).T
            cf[:, C_KVAN + o_i * 2:C_KVAN + o_i * 2 + 2] = inp["kv_a_norm"][o].reshape(2, 128).T
            o_i += 1
    is_b = float(core % 2)
    inv = (10000.0 ** (-np.arange(0, 64, 2, dtype=np.float32) / np.float32(64))).astype(np.float32)
    cf[:, C_INVF] = inv[p % 32]
    cf[:, C_SGN] = np.where((p % 64) < 32, -1.0, 1.0)
    cf[:, C_ISB] = is_b
    cf[:, C_EPS_] = EPS
    gam = 1.0 - 2.0 ** (-5.0 - np.arange(8, dtype=np.float64))
    i = np.arange(128, dtype=np.float64)
    for hp in range(4):
        gh = gam[2 * hp + (p // 64)]
        cf[:, C_G128 + hp] = gh ** 128
        for n in range(16):
            cf[:, C_CFIX + hp * 16 + n] = is_b * gh ** (128 * n)
        cb[:, CB_DQ + hp * 128:CB_DQ + (hp + 1) * 128] = gh[:, None] ** i[None, :]
        cb[:, CB_DK + hp * 128:CB_DK + (hp + 1) * 128] = 0.125 * gh[:, None] ** (-i[None, :])
    jj = np.arange(128)[:, None]
    ii = np.arange(128)[None, :]
    cb[:, CB_MASK:CB_MASK + 128] = (ii >= jj).astype(np.float32)
    cb[:, CB_TRI:CB_TRI + 128] = np.where(ii >= jj, 0.0, NEG)
    cb[:, CB_ID:CB_ID + 128] = np.eye(128, dtype=np.float32)
    return cf, cb


def _layer_weights(li, l, inp):
    d = {}
    if l % 2 == 0:
        e = l // 2
        w = inp["even_w_in"][e]
        sw = _swap_cols(8, 64)
        q, k = w[:, 0:512], w[:, 512:1024]
        v, g = w[:, 1024:2048], w[:, 2048:3072]
        qs, ks = q[:, sw], k[:, sw]
        r = np.empty((4, 128, 8, 1024), np.float32)
        for hp in range(4):
            c2 = slice(hp * 128, (hp + 1) * 128)
            c4 = slice(hp * 256, (hp + 1) * 256)
            blk = np.concatenate([k[:, c2], ks[:, c2], v[:, c4], q[:, c2], qs[:, c2], g[:, c4]], axis=1)
            r[hp] = _kmajor(blk)
        d["w%d_r" % li] = r
        c = np.empty((8, 128, 8, 512), np.float32)
        for i in range(8):
            cs = slice(i * 128, (i + 1) * 128)
            blk = np.concatenate([w[:, 3072:4096][:, cs], w[:, 4096:5120][:, cs], w[:, 5120:6144][:, cs],
                                  w[:, 6144:7168][:, cs]], axis=1)
            c[i] = _kmajor(blk)
        d["w%d_c" % li] = c
        wo = inp["even_w_out"][e]
        d["w%d_o" % li] = np.stack([_kmajor(wo[:, i * 128:(i + 1) * 128]) for i in range(8)])
    else:
        o = l // 2
        w = inp["odd_w_in"][o]
        sw = _swap_cols(1, 64)
        kr = w[:, 640:704]
        d["w%d_i" % li] = _kmajor(np.concatenate([w[:, 0:384], kr, kr[:, sw], w[:, 384:640]], axis=1))
        gate = w[:, 704:1728]
        d["w%d_g" % li] = np.stack([_kmajor(gate[:, h * 128:(h + 1) * 128]) for h in range(8)])
        wq = inp["w_qb"][o]
        wkv = inp["w_kvb"][o]
        qs_, kvs_ = [], []
        for h in range(8):
            b = h * 192
            rp = wq[:, b + 128:b + 192]
            qs_.append(_kmajor(np.concatenate([wq[:, b:b + 128], rp, rp[:, sw]], axis=1)))
            kvs_.append(_kmajor(wkv[:, h * 256:(h + 1) * 256]))
        d["w%d_q" % li] = np.stack(qs_)
        d["w%d_kv" % li] = np.stack(kvs_)
        wo = inp["odd_w_out"][o]
        d["w%d_o" % li] = np.stack([_kmajor(wo[:, i * 128:(i + 1) * 128]) for i in range(8)])
    return d


_PROG_CACHE = {}


def _run(layers, h_cores, inp, use_cc=True, debug=False):
    kinds = tuple("e" if l % 2 == 0 else "o" for l in layers)
    key = (kinds, use_cc, debug)
    if key not in _PROG_CACHE:
        _PROG_CACHE[key] = build_program(list(kinds), use_cc, debug)
    nc = _PROG_CACHE[key]
    shared = {}
    for li, l in enumerate(layers):
        shared.update(_layer_weights(li, l, inp))
    in_maps = []
    for c in range(8):
        b, half = c // 2, c % 2
        pos = inp["positions"][b, half * NT:(half + 1) * NT].astype(np.int32)
        mrow = np.zeros((1, 2 * NT), np.float32)
        mrow[0, :NT] = 1.0 - float(half)
        cf_, cb_ = _const_tables(c, layers, inp)
        m = {"xT": h_cores[c], "pos": np.ascontiguousarray(np.broadcast_to(pos[None, :], (128, NT))),
             "cf": cf_, "cb": cb_, "mrow": mrow}
        m.update(shared)
        in_maps.append(m)
    res = run_bass_kernel_spmd(nc, in_maps, core_ids=list(range(8)))
    if debug:
        return res
    return [np.asarray(res.results[c]["hT_out"]) for c in range(8)]


def kernel(x, positions, pre_norm, post_norm, even_w_in, even_conv_w, even_conv_b, ret_gn, even_w_out, odd_w_in,
           q_a_norm, w_qb, kv_a_norm, w_kvb, odd_w_out):
    inp = dict(x=np.asarray(x, np.float32), positions=np.asarray(positions), pre_norm=np.asarray(pre_norm, np.float32),
               post_norm=np.asarray(post_norm, np.float32), even_w_in=np.asarray(even_w_in, np.float32),
               even_conv_w=np.asarray(even_conv_w, np.float32), even_conv_b=np.asarray(even_conv_b, np.float32),
               ret_gn=np.asarray(ret_gn, np.float32), even_w_out=np.asarray(even_w_out, np.float32),
               odd_w_in=np.asarray(odd_w_in, np.float32), q_a_norm=np.asarray(q_a_norm, np.float32),
               w_qb=np.asarray(w_qb, np.float32), kv_a_norm=np.asarray(kv_a_norm, np.float32),
               w_kvb=np.asarray(w_kvb, np.float32), odd_w_out=np.asarray(odd_w_out, np.float32))
    h = [_feat_major(inp["x"][c // 2, (c % 2) * NT:(c % 2 + 1) * NT, :]) for c in range(8)]
    h = _run(LAUNCHES[0], h, inp) if len(LAUNCHES) == 1 else _multi(h, inp)
    out = np.empty((4, 4096, 1024), np.float32)
    for c in range(8):
        out[c // 2, (c % 2) * NT:(c % 2 + 1) * NT, :] = _from_feat_major(h[c])
    return out


def _multi(h, inp):
    for ls in LAUNCHES:
        h = _run(ls, h, inp)
    return h


LAUNCHES = [[0, 1, 2, 3]]
```

```python
import math
import numpy as np
import concourse.bass as bass
import concourse.mybir as mybir
from concourse.bass_utils import run_bass_kernel_spmd

F32 = mybir.dt.float32
BF16 = mybir.dt.bfloat16
I32 = mybir.dt.int32
AF = mybir.ActivationFunctionType
ALU = mybir.AluOpType

NT = 2048
NG = 4
GS = 512
NCH = 16
EPS = 1e-6
NEG = -30000.0
SCALE = 192.0 ** -0.5
TWO_PI = 2.0 * math.pi
PAIRS = [[0, 1], [2, 3], [4, 5], [6, 7]]

C_PRE = 0
C_POST = 32
C_CW = 64
C_CB = 112
C_GN = 128
C_QAN = 144
C_KVAN = 150
C_INVF = 154
C_SGN = 155
C_ISB = 156
C_G128 = 160
C_CFIX = 164
NCF = 256
CB_TRI = 0
CB_ID = 128
CB_MASK = 256
CB_DQ = 384
CB_DK = 896
NCB = 1408
CB_ONES1 = 1408
CB_ONES128 = 1536
CB_ONES1024 = 1664
NCBF = 1792


class _Op:
    __slots__ = ("eng", "fn", "deps", "kind", "sig", "prev", "need", "sync", "out")

    def __init__(self, eng, fn, deps, kind):
        self.eng, self.fn, self.deps, self.kind = eng, fn, deps, kind
        self.sig = None
        self.prev = None
        self.need = False
        self.sync = []
        self.out = False


class Prog:
    NRING = 8

    def __init__(self, nc):
        self.nc = nc
        self.ops = []
        self.last_w = {}
        self.readers = {}

    def add(self, eng, fn, reads=(), writes=(), kind="c", out=False):
        idx = len(self.ops)
        deps = {}
        for k in reads:
            w = self.last_w.get(k)
            if w is not None:
                deps[w] = True
        for k in writes:
            w = self.last_w.get(k)
            if w is not None:
                deps.setdefault(w, False)
            for r in self.readers.get(k, ()):
                deps.setdefault(r, False)
        for k in reads:
            self.readers.setdefault(k, []).append(idx)
        for k in writes:
            self.last_w[k] = idx
            self.readers[k] = []
        deps.pop(idx, None)
        op = _Op(eng, fn, deps, kind)
        op.out = out
        self.ops.append(op)
        return idx

    def pe(self, fn, r=(), w=()):
        return self.add("pe", fn, r, w)

    def act(self, fn, r=(), w=()):
        return self.add("act", fn, r, w)

    def dve(self, fn, r=(), w=()):
        return self.add("dve", fn, r, w)

    def pool(self, fn, r=(), w=()):
        return self.add("pool", fn, r, w)

    def dma(self, q, fn, r=(), w=(), out=False):
        return self.add(q, fn, r, w, kind="d", out=out)

    def cc(self, fn, r=(), w=()):
        return self.add("pool", fn, r, w, kind="cc")

    @staticmethod
    def _sync_needed(p, c, raw):
        if p.kind != "c" or c.kind != "c":
            return True
        if p.eng != c.eng:
            return True
        if p.eng == "pe":
            return False
        return raw

    def emit(self):
        nc = self.nc
        ops = self.ops
        for op in ops:
            latest = {}
            for d, raw in op.deps.items():
                if self._sync_needed(ops[d], op, raw):
                    if ops[d].kind == "c":
                        pe_ = ops[d].eng
                        if latest.get(pe_, -1) < d:
                            latest[pe_] = d
                    else:
                        ops[d].need = True
                        op.sync.append(d)
            for d in latest.values():
                ops[d].need = True
                op.sync.append(d)
        EPOCH = 3000
        csem = {e: [nc.alloc_semaphore("c_%s0" % e)] for e in ("pe", "act", "dve", "pool")}
        ccnt = {e: 0 for e in csem}
        rings = {q: [nc.alloc_semaphore("d_%s%d" % (q, i)) for i in range(self.NRING)] for q in ("sp", "pool")}
        ruse = {q: [0] * self.NRING for q in rings}
        rpos = {q: 0 for q in rings}
        ccsem = nc.alloc_semaphore("ccsem")
        ccuse = 0
        for op in ops:
            if op.kind == "c":
                if op.need:
                    if ccnt[op.eng] >= EPOCH:
                        csem[op.eng].append(nc.alloc_semaphore("c_%s%d" % (op.eng, len(csem[op.eng]))))
                        ccnt[op.eng] = 0
                    ccnt[op.eng] += 1
                    op.sig = (csem[op.eng][-1], ccnt[op.eng], 1)
            elif op.kind == "d":
                q = op.eng
                i = rpos[q] % self.NRING
                rpos[q] += 1
                if ruse[q][i] > 0:
                    op.prev = (rings[q][i], 16 * ruse[q][i])
                ruse[q][i] += 1
                op.sig = (rings[q][i], 16 * ruse[q][i], 16)
            else:
                if ccuse > 0:
                    op.prev = (ccsem, ccuse)
                ccuse += 1
                op.sig = (ccsem, ccuse, 1)
        engmap = {"pe": "tensor", "act": "scalar", "dve": "vector", "pool": "gpsimd", "sp": "sync"}
        with nc.Block() as block:
            for ename, bname in engmap.items():
                mine = [op for op in ops if op.eng == ename]

                def body(e, mine=mine, ename=ename):
                    seen = {}

                    def wait(s, v):
                        if seen.get(s.num, 0) < v:
                            e.wait_ge(s, v)
                            seen[s.num] = v

                    for op in mine:
                        for d in op.sync:
                            s, v, _ = ops[d].sig
                            wait(s, v)
                        if op.prev is not None:
                            wait(*op.prev)
                        ins = op.fn(e)
                        if op.sig is not None:
                            ins.then_inc(op.sig[0], op.sig[2])
                    if ename == "sp":
                        for op in ops:
                            if op.out:
                                wait(op.sig[0], op.sig[1])

                getattr(block, bname)(body)


DEBUG = []


def build_program(layer_kinds, use_cc=True, debug=False):
    nc = bass.Bass("TRN2", target_bir_lowering=False)
    P = Prog(nc)
    NL = len(layer_kinds)

    def din(name, shape, dt=F32):
        return nc.dram_tensor(name, list(shape), dt, kind="ExternalInput").ap()

    xT_d = din("xT", [128, 8, NT])
    pos_d = din("pos", [128, NT], I32)
    cf_d = din("cf", [128, NCF])
    cb_d = din("cb", [128, NCB])
    mrow_d = din("mrow", [1, 2 * NT])
    W = []
    for li, kd in enumerate(layer_kinds):
        if kd == "e":
            W.append(dict(r=din("w%d_r" % li, [4, 128, 8, 1024]), c=din("w%d_c" % li, [8, 128, 8, 512]),
                          o=din("w%d_o" % li, [8, 128, 16, 128])))
        else:
            W.append(dict(i=din("w%d_i" % li, [128, 8, 768]), g=din("w%d_g" % li, [8, 128, 8, 128]),
                          q=din("w%d_q" % li, [8, 128, 3, 256]), kv=din("w%d_kv" % li, [8, 128, 2, 256]),
                          o=din("w%d_o" % li, [8, 128, 8, 128])))
    out_d = nc.dram_tensor("hT_out", [128, 8, NT], F32, kind="ExternalOutput").ap()
    mixd = nc.dram_tensor("mixd", [16, 128, NT], BF16).ap()
    cu_in = nc.dram_tensor("cu_in", [128, 16], BF16).ap()
    cu_out = nc.dram_tensor("cu_out", [256, 16], BF16).ap()
    ct_in = nc.dram_tensor("ct_in", [128, 128], F32).ap()
    ct_out = nc.dram_tensor("ct_out", [256, 128], F32).ap()
    cl_in = nc.dram_tensor("cl_in", [320, NT], BF16).ap()
    cl_out = nc.dram_tensor("cl_out", [640, NT], BF16).ap()

    hT = nc.alloc_sbuf_tensor("hT", [128, 8, NT], F32)
    uTr = nc.alloc_sbuf_tensor("uTr", [128, 8 * NT], BF16)
    mxr = nc.alloc_sbuf_tensor("mxr", [128, 8 * NT], BF16)
    ar = nc.alloc_sbuf_tensor("ar", [128, 37 * 512], BF16)
    uh_t = nc.alloc_sbuf_tensor("uh_t", [128, 2, 8, 2], BF16)
    cosT = nc.alloc_sbuf_tensor("cosT", [128, NT], BF16)
    sinT = nc.alloc_sbuf_tensor("sinT", [128, NT], BF16)
    NWB = 3
    wb = [nc.alloc_sbuf_tensor("wb%d" % i, [128, 4096], BF16) for i in range(NWB)]
    cf = nc.alloc_sbuf_tensor("cf_sb", [128, NCF], F32)
    cbf = nc.alloc_sbuf_tensor("cbf", [128, NCBF], BF16)
    sqb = [nc.alloc_sbuf_tensor("sqb%d" % i, [128, 512], BF16) for i in range(2)]
    rsb = nc.alloc_sbuf_tensor("rsb", [128, 512], F32)
    scr = nc.alloc_sbuf_tensor("scr", [128, 8], F32)
    onesf = nc.alloc_sbuf_tensor("onesf", [128, 128], F32)
    PS = [nc.alloc_psum_tensor("ps%d" % i, [128, 512], F32) for i in range(8)]

    def view(reg, off, shape, dt=BF16, p0=0, p1=128):
        n = 1
        for s in shape[1:]:
            n *= s
        if dt == BF16:
            a = reg[p0:p1, off // 2: off // 2 + n]
        else:
            a = reg[p0:p1, off // 2: off // 2 + 2 * n].bitcast(dt)
        if len(shape) == 3:
            a = a.rearrange("p (a b) -> p a b", a=shape[1])
        return a

    uT = view(uTr, 0, [128, 8, NT])
    tri = cbf[:, CB_TRI:CB_TRI + 128]
    ident = cbf[:, CB_ID:CB_ID + 128]
    ones1 = cbf[:, CB_ONES1:CB_ONES1 + 128]
    ones128 = cbf[:, CB_ONES128:CB_ONES128 + 128]
    ones1024 = cbf[:, CB_ONES1024:CB_ONES1024 + 128]

    def col(c, p0=0, p1=128):
        return cf[p0:p1, c:c + 1]

    K = 1024

    def mm(out, lhsT, rhs, start, stop):
        return lambda e: e.matmul(out, lhsT, rhs, start=start, stop=stop)

    def tt(out, a, b, op):
        return lambda e: e.tensor_tensor(out, a, b, op)

    def ts(out, a, s1, s2, op0, op1=None):
        if op1 is None:
            return lambda e: e.tensor_scalar(out, a, s1, None, op0)
        return lambda e: e.tensor_scalar(out, a, s1, s2, op0, op1)

    def stt(out, a, s, b, op0, op1):
        return lambda e: e.scalar_tensor_tensor(out, a, s, b, op0, op1)

    def actf(out, in_, func, bias=None, scale=None):
        kw = {}
        if bias is not None:
            kw["bias"] = bias
        if scale is not None:
            kw["scale"] = scale
        return lambda e: e.activation(out, in_, func, **kw)

    def cp(out, in_):
        return lambda e: e.tensor_copy(out, in_)

    def dmaf(out, in_):
        return lambda e: e.dma_start(out=out, in_=in_)

    bar_n = [0]

    def dbg(name, ap, keys):
        if not debug:
            return
        shp = list(ap.shape)
        d_ = nc.dram_tensor("dbg_" + name, shp, ap.dtype, kind="ExternalOutput").ap()
        DEBUG.append("dbg_" + name)
        P.dma("sp", dmaf(d_, ap), r=keys, out=True)

    def barrier(region_keys):
        bar_n[0] += 1
        P.dve(lambda e: e.memset(scr[:, 0:1], 0.0), r=(), w=list(region_keys) + [("scr",)])

    wplan = []
    for li_, kd_ in enumerate(layer_kinds):
        if kd_ == "e":
            for hp_ in range(4):
                wplan.append([W[li_]["r"][hp_][:, :, 0:512]])
                wplan.append([W[li_]["r"][hp_][:, :, 512:1024]])
            for i_ in range(8):
                wplan.append([W[li_]["c"][i_]])
        else:
            wplan.append([W[li_]["i"][:, :, 0:512]])
            wplan.append([W[li_]["i"][:, :, 512:768]])
            for h_ in range(8):
                wplan.append([W[li_]["q"][h_], W[li_]["kv"][h_], W[li_]["g"][h_]])
    wstate = {"issued": 0, "used": 0, "views": {}}

    def _issue(j):
        i = j % NWB
        key = ("wb", i)
        views = []
        off = 0
        for src in wplan[j]:
            a, b = src.shape[1], src.shape[2]
            v = wb[i][:, off:off + a * b].rearrange("p (a b) -> p a b", a=a)
            P.dma("pool", dmaf(v, src), r=(), w=[key])
            views.append(v)
            off += a * b
        assert off <= 4096
        wstate["views"][j] = (views, key)

    def wload(src_aps):
        j = wstate["used"]
        assert len(src_aps) == len(wplan[j])
        while wstate["issued"] < min(len(wplan), j + NWB):
            _issue(wstate["issued"])
            wstate["issued"] += 1
        wstate["used"] += 1
        return wstate["views"].pop(j)

    P.dma("sp", dmaf(cf[:], cf_d), w=[("cf",)])
    for q in range(4):
        P.dma("sp", dmaf(hT[:, 2 * q:2 * q + 2, :], xT_d[:, 2 * q:2 * q + 2, :]), w=[("hT", 2 * q), ("hT", 2 * q + 1)])
    posi = view(mxr, 0, [128, NT], I32)
    ang = view(mxr, 8192, [128, NT], F32)
    t_a = view(mxr, 16384, [128, NT], F32)
    t_n = view(mxr, 24576, [128, NT], I32)
    t_b = view(ar, 0, [128, NT], F32)
    t_c = view(ar, 8192, [128, NT], F32)
    P.dma("sp", dmaf(posi, pos_d), w=[("posi",)])
    cbst = view(ar, 16384, [128, NCB], F32)
    P.dma("sp", dmaf(cbst, cb_d), w=[("cbst",)])
    P.dve(cp(cbf[:, 0:NCB], cbst), r=[("cbst",)], w=[("cbf",)])
    P.pool(lambda e: e.memset(onesf[:], 1.0), w=[("cbf1",)])
    P.pool(lambda e: e.memset(ones1, 1.0), w=[("cbf1",)])
    P.pool(lambda e: e.memset(ones128, 1.0 / 128), w=[("cbf1",)])
    P.pool(lambda e: e.memset(ones1024, 1.0 / 1024), w=[("cbf1",)])
    P.dve(lambda e: e.memset(scr[:, 1:2], 0.0), r=[("cbf1",)], w=[("cbf",)])
    P.dve(cp(ang, posi), r=[("posi",)], w=[("ang",)])
    P.dve(ts(ang, ang, col(C_INVF), None, ALU.mult), r=[("ang",), ("cf",)], w=[("ang",)])
    for which, shift, dst in (("s", 0.0, sinT), ("c", 0.5 * math.pi, cosT)):
        P.dve(ts(t_a, ang, shift, 1.0 / TWO_PI, ALU.add, ALU.mult), r=[("ang",)], w=[("t_a",)])
        P.dve(cp(t_n, t_a), r=[("t_a",)], w=[("t_n",)])
        P.dve(cp(t_b, t_n), r=[("t_n",)], w=[("t_b",)])
        P.dve(ts(t_a, ang, shift, None, ALU.add), r=[("ang",), ("t_n",)], w=[("t_a",)])
        P.dve(stt(t_b, t_b, -TWO_PI, t_a, ALU.mult, ALU.add), r=[("t_b",), ("t_a",)], w=[("t_b",)])
        P.dve(ts(t_c, t_b, math.pi, -TWO_PI, ALU.is_gt, ALU.mult), r=[("t_b",)], w=[("t_c",)])
        P.dve(tt(t_b, t_b, t_c, ALU.add), r=[("t_b",), ("t_c",)], w=[("t_b",)])
        P.dve(ts(t_c, t_b, -math.pi, TWO_PI, ALU.is_lt, ALU.mult), r=[("t_b",)], w=[("t_c",)])
        P.dve(tt(t_b, t_b, t_c, ALU.add), r=[("t_b",), ("t_c",)], w=[("t_b",)])
        P.dve(ts(t_b, t_b, math.pi, -math.pi, ALU.min, ALU.max), r=[("t_b",)], w=[("t_b",)])
        if which == "s":
            P.act(actf(t_c, t_b, AF.Sin), r=[("t_b",)], w=[("t_c",)])
            P.dve(ts(dst[:], t_c, col(C_SGN), None, ALU.mult), r=[("t_c",), ("cf",)], w=[("rope",)])
        else:
            P.act(actf(dst[:], t_b, AF.Sin), r=[("t_b",)], w=[("rope",)])

    REG_U = [("Ru",)]
    REG_M = [("Rm",)]
    REG_A = [("Ra",)]
    barrier(REG_U + REG_M + REG_A + [("t_a",), ("t_b",), ("t_c",), ("t_n",), ("ang",), ("posi",), ("cbst",)])

    def rstd_from(psn, scale, rkeys):
        P.act(actf(rsb[:], psn, AF.Sqrt, bias=col(C_EPS_), scale=scale), r=rkeys + [("cf",)], w=[("rsb",)])
        P.dve(lambda e: e.reciprocal(rsb[:], rsb[:]), r=[("rsb",)], w=[("rsb",)])

    def prenorm(l):
        for g in range(NG):
            tok = slice(g * GS, (g + 1) * GS)
            for kc in range(8):
                s = sqb[kc % 2]
                P.act(actf(s[:], hT[:, kc, tok], AF.Square), r=[("hT", kc)], w=[("sqb", kc % 2)])
                P.pe(mm(PS[6][:], ones1024, s[:], kc == 0, kc == 7), r=[("sqb", kc % 2), ("cbf",)], w=[("ps", 6)])
            rstd_from(PS[6][:], 1.0, [("ps", 6)])
            for kc in range(8):
                P.dve(stt(uT[:, kc, tok], hT[:, kc, tok], col(C_PRE + l * 8 + kc), rsb[:], ALU.mult, ALU.mult),
                      r=[("hT", kc), ("rsb",), ("cf",)] + REG_U, w=[("uT", kc, g)])

    def outproj(l, nf, wo_d, post_col):
        barrier(REG_U)
        barrier(REG_M)
        wo = view(mxr, 0, [128, nf, 1024])
        yT = view(uTr, 0, [128, 8, 512], F32)
        nmb = 2 if nf == 8 else 1
        mixgs = [view(uTr, 16384 + 8192 * i, [128, nf, 512]) for i in range(nmb)]
        for cc_ in range(8):
            P.dma("pool", dmaf(wo[:, :, cc_ * 128:(cc_ + 1) * 128], wo_d[cc_]), r=REG_M, w=[("wo", cc_)])
        for g in range(NG):
            tok = slice(g * GS, (g + 1) * GS)
            mixg = mixgs[g % nmb]
            mgk = ("mixg", g % nmb)
            P.dma("sp", dmaf(mixg, mixd[0:nf, :, tok].rearrange("f p t -> p f t")),
                  r=REG_U + [("mixd", f) for f in range(nf)], w=[mgk])
            for cc_ in range(8):
                ps = PS[cc_ % 2]
                pk = ("ps", cc_ % 2)
                for f in range(nf):
                    P.pe(mm(ps[:], wo[:, f, cc_ * 128:(cc_ + 1) * 128], mixg[:, f, :], f == 0, f == nf - 1),
                         r=[("wo", cc_), mgk], w=[pk])
                P.act(actf(yT[:, cc_, :], ps[:], AF.Copy), r=[pk] + REG_U, w=[("yT", cc_)])
                s = sqb[cc_ % 2]
                P.act(actf(s[:], ps[:], AF.Square), r=[pk], w=[("sqb", cc_ % 2)])
                P.pe(mm(PS[6][:], ones1024, s[:], cc_ == 0, cc_ == 7), r=[("sqb", cc_ % 2), ("cbf",)], w=[("ps", 6)])
            rstd_from(PS[6][:], 1.0, [("ps", 6)])
            for cc_ in range(8):
                P.dve(stt(yT[:, cc_, :], yT[:, cc_, :], col(post_col + cc_), rsb[:], ALU.mult, ALU.mult),
                      r=[("yT", cc_), ("rsb",), ("cf",)], w=[("yT", cc_)])
            for cc_ in range(8):
                P.dve(tt(hT[:, cc_, tok], hT[:, cc_, tok], yT[:, cc_, :], ALU.add), r=[("yT", cc_), ("hT", cc_)] + REG_U,
                      w=[("hT", cc_)])
        barrier(REG_U)
        barrier(REG_M)

    def exchange(src_list, cin, cout, dst_list, keys_r, keys_w, nrows):
        for rows, sap in src_list:
            P.dma("sp", dmaf(cin[rows], sap), r=keys_r, w=[("cin", id(cin))])
        if use_cc:
            P.cc(lambda e: e.collective_compute("AllGather", ALU.bypass, replica_groups=PAIRS, ins=[cin], outs=[cout]),
                 r=[("cin", id(cin))], w=[("cout", id(cout))])
        else:
            P.dma("sp", dmaf(cout[0:nrows], cin), r=[("cin", id(cin))], w=[("cout", id(cout))])
        for sap, rows in dst_list:
            P.dma("sp", dmaf(sap, cout[rows]), r=[("cout", id(cout))] + REG_A + REG_M, w=keys_w)

    def even_layer(l, e_i, Wl):
        prenorm(l)
        uh = uh_t[:, 0, :, :]
        uh_own = uh_t[:, 1, :, :]
        for kc in range(8):
            P.dve(cp(uh_own[:, kc, :], uT[:, kc, NT - 2:NT]), r=[("uT", kc, 3)] + REG_A, w=[("uh_own",)])
        exchange([(slice(0, 128), uh_own.rearrange("p a b -> p (a b)"))], cu_in, cu_out,
                 [(uh.rearrange("p a b -> p (a b)"), slice(0, 128))], [("uh_own",)], [("uh",)], 128)

        qT = view(mxr, 0, [128, NT])
        kT = view(mxr, 4096, [128, NT])
        Kt = view(mxr, 8192, [128, NCH, 128])
        V = view(mxr, 12288, [128, NCH, 256])
        sg = view(mxr, 20480, [128, 2, NT])
        Tb = view(mxr, 28672, [128, NCH, 128])
        Tf = view(ar, 0, [128, 17, 128], F32)
        Tin = view(ar, 8704, [128, 128], F32)
        t1 = view(ar, 9216, [128, 512], F32)
        t2 = view(ar, 11264, [128, 512], F32)
        PT = [view(ar, 13312 + 256 * i, [128, 128]) for i in range(4)]
        stage = [view(ar, 14336 + 4096 * i, [128, NT]) for i in range(2)]
        zb = view(ar, 22528, [128, 2 + NT], F32)
        cxs = view(ar, 30752, [128, 512], F32)
        acc = view(ar, 32800, [128, 512], F32)
        sgc = view(ar, 34848, [128, 512])
        zh = view(ar, 35872, [128, 2], F32)
        mask01 = cbf[:, CB_MASK:CB_MASK + 128]
        gam = [1.0 - 2.0 ** (-5.0 - h) for h in range(8)]

        def rope_evac(psa, psb, g, dst, dec, pk_a, pk_b, dkey):
            tok = slice(g * GS, (g + 1) * GS)
            P.dve(tt(t1, psa, cosT[:, tok], ALU.mult), r=[pk_a, ("rope",)] + REG_A, w=[("t1",)])
            P.dve(tt(t2, psb, sinT[:, tok], ALU.mult), r=[pk_b, ("rope",)] + REG_A, w=[("t2",)])
            P.pool(tt(t1, t1, t2, ALU.add), r=[("t1",), ("t2",)], w=[("t1",)])
            P.dve(tt(dst[:, tok].rearrange("p (c i) -> p c i", c=4), t1.rearrange("p (c i) -> p c i", c=4),
                     dec.unsqueeze(1).to_broadcast([128, 4, 128]), ALU.mult),
                  r=[("t1",), ("cbf",)] + REG_M, w=[dkey])

        for hp in range(4):
            (wA,), wk_key = wload([Wl["r"][hp][:, :, 0:512]])
            for g in range(NG):
                tok = slice(g * GS, (g + 1) * GS)
                pb = 4 * (g % 2)
                for j, pi in ((0, pb), (1, pb + 1)):
                    for kc in range(8):
                        P.pe(mm(PS[pi][:], wA[:, kc, j * 128:(j + 1) * 128], uT[:, kc, tok], kc == 0, kc == 7),
                             r=[wk_key, ("uT", kc, g)], w=[("ps", pi)])
                rope_evac(PS[pb][:], PS[pb + 1][:], g, kT, cbf[:, CB_DK + hp * 128:CB_DK + (hp + 1) * 128],
                          ("ps", pb), ("ps", pb + 1), ("kT", g))
                for c in range(4 * g, 4 * g + 4):
                    pT = PS[7][:, 0:64].bitcast(BF16)
                    P.pe(lambda e, c=c, pT=pT: e.transpose(pT, kT[:, c * 128:(c + 1) * 128], ident),
                         r=[("kT", g), ("cbf",)], w=[("ps", 7)])
                    for hh in range(2):
                        gv = gam[2 * hp + hh] ** 128
                        P.act(actf(Kt[:, c, hh * 64:(hh + 1) * 64], pT[:, hh * 64:(hh + 1) * 64], AF.Copy, scale=gv),
                              r=[("ps", 7)] + REG_M, w=[("Kt", c)])
                    pv = PS[2 + (c % 2)]
                    for kc in range(8):
                        P.pe(mm(pv[:, 0:256], uT[:, kc, c * 128:(c + 1) * 128], wA[:, kc, 256:512], kc == 0, kc == 7),
                             r=[wk_key, ("uT", kc, g)], w=[("ps", 2 + (c % 2))])
                    P.act(actf(V[:, c, :], pv[:, 0:256], AF.Copy), r=[("ps", 2 + (c % 2))] + REG_M, w=[("V", c)])
            P.dve(lambda e: e.memset(Tf[:, 0, :], 0.0), r=REG_A, w=[("Tf", 0)])
            for c in range(NCH):
                for hh in range(2):
                    rows = slice(64 * hh, 64 * hh + 64)
                    pk = ("ps", 4 + hh)
                    P.pe(mm(PS[4 + hh][:, 0:128], Kt[:, c, :], V[:, c, hh * 128:(hh + 1) * 128], True, True),
                         r=[("Kt", c), ("V", c)], w=[pk])
                    P.dve(stt(Tf[rows, c + 1, :], Tf[rows, c, :], col(C_G128 + hp, 64 * hh, 64 * hh + 64),
                              PS[4 + hh][rows, 0:128], ALU.mult, ALU.add),
                          r=[pk, ("Tf", c), ("cf",)], w=[("Tf", c + 1)])
            exchange([(slice(0, 128), Tf[:, 16, :])], ct_in, ct_out, [(Tin, slice(0, 128))], [("Tf", 16)], [("Tin",)], 128)

            (wB,), wq_key = wload([Wl["r"][hp][:, :, 512:1024]])
            for g in range(NG):
                tok = slice(g * GS, (g + 1) * GS)
                pb = 4 * (g % 2)
                for j, pi in ((0, pb), (1, pb + 1)):
                    for kc in range(8):
                        P.pe(mm(PS[pi][:], wB[:, kc, j * 128:(j + 1) * 128], uT[:, kc, tok], kc == 0, kc == 7),
                             r=[wq_key, ("uT", kc, g)], w=[("ps", pi)])
                rope_evac(PS[pb][:], PS[pb + 1][:], g, qT, cbf[:, CB_DQ + hp * 128:CB_DQ + (hp + 1) * 128],
                          ("ps", pb), ("ps", pb + 1), ("qT", g))
                for hh in range(2):
                    gi = pb + 2 + hh
                    for kc in range(8):
                        P.pe(mm(PS[gi][:], wB[:, kc, 256 + hh * 128:256 + (hh + 1) * 128], uT[:, kc, tok],
                                kc == 0, kc == 7), r=[wq_key, ("uT", kc, g)], w=[("ps", gi)])
                    P.act(actf(sg[:, hh, tok], PS[gi][:], AF.Silu), r=[("ps", gi)] + REG_M, w=[("sg", hh, g)])
            for n in range(NCH):
                P.dve(stt(Tb[:, n, :], Tin, col(C_CFIX + hp * 16 + n), Tf[:, n, :], ALU.mult, ALU.add),
                      r=[("Tin",), ("Tf", n), ("cf",)] + REG_M, w=[("Tb", n)])
            def emit_ST(c):
                g = c // 4
                cs = slice(c * 128, (c + 1) * 128)
                for hh in range(2):
                    rows = slice(64 * hh, 64 * hh + 64)
                    si = hh + 4 * (c % 2)
                    P.pe(mm(PS[si][:, 0:128], kT[rows, cs], qT[rows, cs], True, True), r=[("kT", g), ("qT", g)],
                         w=[("ps", si)])

            emit_ST(0)
            for c in range(NCH):
                g, cl = c // 4, c % 4
                cs = slice(c * 128, (c + 1) * 128)
                if c + 1 < NCH:
                    emit_ST(c + 1)
                for hh in range(2):
                    rows = slice(64 * hh, 64 * hh + 64)
                    si = hh + 4 * (c % 2)
                    pS = PS[si]
                    pR = PS[2 + hh]
                    pt = PT[2 * hh + (c % 2)]
                    ptk = ("PT", 2 * hh + (c % 2))
                    P.dve(tt(pt, pS[:, 0:128], mask01, ALU.mult), r=[("ps", si), ("cbf",)] + REG_A, w=[ptk])
                    P.pe(mm(pR[:, cl * 128:(cl + 1) * 128], V[:, c, hh * 128:(hh + 1) * 128], pt, True, False),
                         r=[("V", c), ptk], w=[("ps", 2 + hh)])
                    P.pe(mm(pR[:, cl * 128:(cl + 1) * 128], Tb[rows, c, :], qT[rows, cs], False, True),
                         r=[("Tb", c), ("qT", g)], w=[("ps", 2 + hh)])
                if cl == 3:
                    tok = slice(g * GS, (g + 1) * GS)
                    for hh in range(2):
                        pR = PS[2 + hh]
                        s = sqb[hh]
                        P.act(actf(s[:], pR[:], AF.Square), r=[("ps", 2 + hh)], w=[("sqb", hh)])
                        P.pe(mm(PS[6][:], ones128, s[:], True, True), r=[("sqb", hh), ("cbf",)], w=[("ps", 6)])
                        rstd_from(PS[6][:], 1.0, [("ps", 6)])
                        P.dve(stt(t1, pR[:], col(C_GN + e_i * 8 + 2 * hp + hh), rsb[:], ALU.mult, ALU.mult),
                              r=[("ps", 2 + hh), ("rsb",), ("cf",)] + REG_A, w=[("t1",)])
                        P.pool(tt(stage[hh][:, tok], t1, sg[:, hh, tok], ALU.mult), r=[("t1",), ("sg", hh, g)],
                               w=[("stage", hh)])
            for hh in range(2):
                P.dma("sp", dmaf(mixd[2 * hp + hh], stage[hh]), r=[("stage", hh)], w=[("mixd", 2 * hp + hh)])

        for i in range(8):
            (wC,), wc_key = wload([Wl["c"][i]])
            pH = PS[4]
            for j, cb_ in ((0, 128), (1, 256)):
                for kc in range(8):
                    P.pe(mm(pH[:, 2 * j:2 * j + 2], wC[:, kc, cb_:cb_ + 128], uh[:, kc, :], kc == 0, kc == 7),
                         r=[wc_key, ("uh",)], w=[("ps", 4)])
            P.act(actf(zh, pH[:, 2:4], AF.Copy), r=[("ps", 4)] + REG_A, w=[("zh",)])
            P.dve(stt(zb[:, 0:2], pH[:, 0:2], col(C_ISB), zh, ALU.mult, ALU.mult), r=[("ps", 4), ("zh",), ("cf",)],
                  w=[("zb", -1)])
            st = stage[i % 2]
            for g in range(NG):
                tok = slice(g * GS, (g + 1) * GS)
                pb = 4 * (g % 2)
                for j in (2, 1, 3, 0):
                    for kc in range(8):
                        P.pe(mm(PS[pb + j][:], wC[:, kc, j * 128:(j + 1) * 128], uT[:, kc, tok], kc == 0, kc == 7),
                             r=[wc_key, ("uT", kc, g)], w=[("ps", pb + j)])
                P.act(actf(cxs, PS[pb + 2][:], AF.Copy), r=[("ps", pb + 2)] + REG_A, w=[("cxs",)])
                P.act(actf(sgc, PS[pb + 3][:], AF.Silu), r=[("ps", pb + 3)] + REG_A, w=[("sgc",)])
                P.dve(tt(zb[:, 2 + g * GS:2 + (g + 1) * GS], PS[pb + 1][:], cxs, ALU.mult), r=[("ps", pb + 1), ("cxs",)],
                      w=[("zb", g)])
                zr = [("zb", g), ("zb", g - 1), ("cf",)]
                cw = C_CW + e_i * 24 + i
                P.dve(ts(acc, zb[:, g * GS:g * GS + GS], col(cw), None, ALU.mult), r=zr, w=[("acc",)])
                P.dve(stt(acc, zb[:, g * GS + 1:g * GS + 1 + GS], col(cw + 8), acc, ALU.mult, ALU.add),
                      r=zr + [("acc",)], w=[("acc",)])
                P.dve(stt(acc, zb[:, g * GS + 2:g * GS + 2 + GS], col(cw + 16), acc, ALU.mult, ALU.add),
                      r=zr + [("acc",)], w=[("acc",)])
                P.dve(stt(acc, acc, col(C_CB + e_i * 8 + i), PS[pb][:], ALU.add, ALU.mult),
                      r=[("acc",), ("ps", pb), ("cf",)], w=[("acc",)])
                P.pool(tt(st[:, tok], acc, sgc, ALU.mult), r=[("acc",), ("sgc",)], w=[("stage", i % 2)])
            P.dma("sp", dmaf(mixd[8 + i], st), r=[("stage", i % 2)], w=[("mixd", 8 + i)])

        outproj(l, 16, Wl["o"], C_POST + l * 8)
        barrier(REG_A)

    def odd_layer(l, o_i, Wl):
        prenorm(l)
        latT = view(mxr, 0, [128, 2, 2 * NT])
        krT = view(mxr, 16384, [128, 2 * NT])
        knT = view(mxr, 24576, [128, 2 * NT])
        cqT = view(ar, 0, [128, 3, NT])
        Vv = view(ar, 12288, [128, 32, 128])
        qnT = view(ar, 20480, [128, NT])
        qrT = view(ar, 24576, [128, NT])
        PTs = [view(ar, 28672 + 1024 * i, [128, 512]) for i in range(2)] + [view(ar, 36864, [128, 512])]
        sgh = view(ar, 30720, [128, NT])
        t1 = view(ar, 34816, [128, 512], F32)
        t2 = view(ar, 36864, [128, 256], F32)
        (wI,), wi_key = wload([Wl["i"][:, :, 0:512]])
        P.pool(lambda e: e.memset(krT[64:96, :], 0.0), r=REG_M, w=[("krT", "m")])
        P.dma("pool", dmaf(krT[64:65, :], mrow_d), r=REG_M, w=[("krT", "m")])
        P.pool(lambda e: e.memset(qrT[64:96, :], 0.0), r=REG_A, w=[("qrT", "m")])
        P.pool(lambda e: e.memset(qrT[64:65, :], NEG), r=REG_A, w=[("qrT", "m")])
        for g in range(NG):
            tok = slice(g * GS, (g + 1) * GS)
            otok = slice(NT + g * GS, NT + (g + 1) * GS)
            for j in range(3):
                for kc in range(8):
                    P.pe(mm(PS[j][:], wI[:, kc, j * 128:(j + 1) * 128], uT[:, kc, tok], kc == 0, kc == 7),
                         r=[wi_key, ("uT", kc, g)], w=[("ps", j)])
                s = sqb[j % 2]
                P.act(actf(s[:], PS[j][:], AF.Square), r=[("ps", j)], w=[("sqb", j % 2)])
                P.pe(mm(PS[6][:], ones128, s[:], j == 0, j == 2), r=[("sqb", j % 2), ("cbf",)], w=[("ps", 6)])
            rstd_from(PS[6][:], 1.0 / 3.0, [("ps", 6)])
            for j in range(3):
                P.dve(stt(cqT[:, j, tok], PS[j][:], col(C_QAN + o_i * 3 + j), rsb[:], ALU.mult, ALU.mult),
                      r=[("ps", j), ("rsb",), ("cf",)] + REG_A, w=[("cqT", g)])
            for j, pi in ((0, 3), (1, 4)):
                for kc in range(8):
                    P.pe(mm(PS[pi][0:64, :], wI[:, kc, 384 + 64 * j:448 + 64 * j], uT[:, kc, tok], kc == 0, kc == 7),
                         r=[wi_key, ("uT", kc, g)], w=[("ps", pi)])
            P.dve(tt(t1[0:64, :], PS[3][0:64, :], cosT[0:64, tok], ALU.mult), r=[("ps", 3), ("rope",)] + REG_A, w=[("t1",)])
            P.dve(tt(rsb[0:64, :], PS[4][0:64, :], sinT[0:64, tok], ALU.mult), r=[("ps", 4), ("rope",), ("rsb",)],
                  w=[("rsb",)])
            P.pool(tt(krT[0:64, otok], t1[0:64, :], rsb[0:64, :], ALU.add), r=[("t1",), ("rsb",)] + REG_M,
                   w=[("krT", 4 + g)])
        (wI2,), wi2_key = wload([Wl["i"][:, :, 512:768]])
        for g in range(NG):
            tok = slice(g * GS, (g + 1) * GS)
            otok = slice(NT + g * GS, NT + (g + 1) * GS)
            for j in range(2):
                for kc in range(8):
                    P.pe(mm(PS[j][:], wI2[:, kc, j * 128:(j + 1) * 128], uT[:, kc, tok], kc == 0, kc == 7),
                         r=[wi2_key, ("uT", kc, g)], w=[("ps", j)])
                s = sqb[j % 2]
                P.act(actf(s[:], PS[j][:], AF.Square), r=[("ps", j)], w=[("sqb", j % 2)])
                P.pe(mm(PS[6][:], ones128, s[:], j == 0, j == 1), r=[("sqb", j % 2), ("cbf",)], w=[("ps", 6)])
            rstd_from(PS[6][:], 0.5, [("ps", 6)])
            for j in range(2):
                P.dve(stt(latT[:, j, otok], PS[j][:], col(C_KVAN + o_i * 2 + j), rsb[:], ALU.mult, ALU.mult),
                      r=[("ps", j), ("rsb",), ("cf",)] + REG_M, w=[("latT", 4 + g)])
        own = slice(NT, 2 * NT)
        prv = slice(0, NT)
        lk = [("latT", 4 + g) for g in range(NG)] + [("krT", 4 + g) for g in range(NG)]
        pk_ = [("latT", g) for g in range(NG)] + [("krT", g) for g in range(NG)]
        exchange([(slice(0, 128), latT[:, 0, own]), (slice(128, 256), latT[:, 1, own]), (slice(256, 320), krT[0:64, own])],
                 cl_in, cl_out,
                 [(latT[:, 0, prv], slice(0, 128)), (latT[:, 1, prv], slice(128, 256)), (krT[0:64, prv], slice(256, 320))],
                 lk, pk_, 320)
        allk = [("latT", g) for g in range(8)]
        allkr = [("krT", g) for g in range(8)] + [("krT", "m")]

        for h in range(8):
            (wq, wkv, wg), wh_key = wload([Wl["q"][h], Wl["kv"][h], Wl["g"][h]])
            for kg in range(8):
                ks = slice(kg * 512, (kg + 1) * 512)
                pi = kg % 2
                for kc in range(2):
                    P.pe(mm(PS[pi][:], wkv[:, kc, 0:128], latT[:, kc, ks], kc == 0, kc == 1),
                         r=[wh_key, ("latT", kg)], w=[("ps", pi)])
                if kg % 2 == 0:
                    P.act(actf(knT[:, ks], PS[pi][:], AF.Copy), r=[("ps", pi)] + REG_M, w=[("knT", kg)])
                else:
                    P.dve(cp(knT[:, ks], PS[pi][:]), r=[("ps", pi)] + REG_M, w=[("knT", kg)])
            for kg in range(8):
                pi = 2 + kg % 2
                for j in range(4):
                    kb = 4 * kg + j
                    for kc in range(2):
                        P.pe(mm(PS[pi][:, j * 128:(j + 1) * 128], latT[:, kc, kb * 128:(kb + 1) * 128], wkv[:, kc, 128:256],
                                kc == 0, kc == 1), r=[wh_key, ("latT", kg)], w=[("ps", pi)])
                dstv = Vv[:, 4 * kg:4 * kg + 4, :].rearrange("p a b -> p (a b)")
                if kg % 2 == 0:
                    P.dve(cp(dstv, PS[pi][:]), r=[("ps", pi)] + REG_A, w=[("Vv", kg)])
                else:
                    P.act(actf(dstv, PS[pi][:], AF.Copy), r=[("ps", pi)] + REG_A, w=[("Vv", kg)])
            for g in range(NG):
                tok = slice(g * GS, (g + 1) * GS)
                for kc in range(3):
                    P.pe(mm(PS[0][:], wq[:, kc, 0:128], cqT[:, kc, tok], kc == 0, kc == 2),
                         r=[wh_key, ("cqT", g)], w=[("ps", 0)])
                P.act(actf(qnT[:, tok], PS[0][:], AF.Copy, scale=SCALE), r=[("ps", 0)] + REG_A, w=[("qnT", g)])
                for j, pi in ((0, 1), (1, 2)):
                    for kc in range(3):
                        P.pe(mm(PS[pi][0:64, :], wq[:, kc, 128 + 64 * j:192 + 64 * j], cqT[:, kc, tok], kc == 0, kc == 2),
                             r=[wh_key, ("cqT", g)], w=[("ps", pi)])
                P.dve(stt(t1[0:64, :], PS[1][0:64, :], SCALE, cosT[0:64, tok], ALU.mult, ALU.mult),
                      r=[("ps", 1), ("rope",)] + REG_A, w=[("t1",)])
                P.dve(stt(rsb[0:64, :], PS[2][0:64, :], SCALE, sinT[0:64, tok], ALU.mult, ALU.mult),
                      r=[("ps", 2), ("rope",), ("rsb",)], w=[("rsb",)])
                P.pool(tt(qrT[0:64, tok], t1[0:64, :], rsb[0:64, :], ALU.add), r=[("t1",), ("rsb",)] + REG_A,
                       w=[("qrT", g)])
                for kc in range(8):
                    P.pe(mm(PS[3][:], wg[:, kc, :], uT[:, kc, tok], kc == 0, kc == 7), r=[wh_key, ("uT", kc, g)],
                         w=[("ps", 3)])
                P.act(actf(sgh[:, tok], PS[3][:], AF.Silu), r=[("ps", 3)] + REG_A, w=[("sgh", g)])
            if h == 0:
                dbg("cqT", cqT.rearrange("p a b -> p (a b)"), [("cqT", g) for g in range(NG)])
                dbg("latT", latT.rearrange("p a b -> p (a b)"), allk)
                dbg("krT", krT[0:96, :], allkr)
                dbg("knT", knT, [("knT", kg) for kg in range(8)])
                dbg("Vv", Vv.rearrange("p a b -> p (a b)"), [("Vv", kg) for kg in range(8)])
                dbg("qnT", qnT, [("qnT", g) for g in range(NG)])
                dbg("qrT", qrT[0:96, :], [("qrT", g) for g in range(NG)] + [("qrT", "m")])
                dbg("gate", sgh, [("sgh", g) for g in range(NG)])
            knk = [("knT", kg) for kg in range(8)]
            vk = [("Vv", kg) for kg in range(8)]
            for qg in range(NG):
                qtok0 = qg * GS
                blocks = [(kb, 0) for kb in range(16 + 4 * qg)] + [(16 + 4 * qg + t, t) for t in range(4)]
                nb = len(blocks)

                def emit_S(bi):
                    kb, t = blocks[bi]
                    diag = kb >= 16 + 4 * qg
                    q0 = qtok0 + 128 * t
                    n = GS - 128 * t
                    pS = PS[3 + bi % 3]
                    pk = ("ps", 3 + bi % 3)
                    ks = slice(kb * 128, (kb + 1) * 128)
                    P.pe(mm(pS[:, 0:n], knT[:, ks], qnT[:, q0:q0 + n], True, False), r=knk + [("qnT", qg)], w=[pk])
                    P.pe(mm(pS[:, 0:n], krT[0:96, ks], qrT[0:96, q0:q0 + n], False, not diag),
                         r=allkr + [("qrT", qg), ("qrT", "m")], w=[pk])
                    if diag:
                        P.pe(mm(pS[:, 0:128], ident, tri, False, True), r=[("cbf",)], w=[pk])

                def emit_PV(bi):
                    kb, t = blocks[bi]
                    n = GS - 128 * t
                    pS = PS[3 + bi % 3]
                    pk = ("ps", 3 + bi % 3)
                    pt = PTs[bi % 3]
                    P.act(actf(pt[:, 0:n], pS[:, 0:n], AF.Exp), r=[pk] + REG_A, w=[("PTs", bi % 3)])
                    P.pe(mm(PS[6][:, 128 * t:GS], Vv[:, kb, :], pt[:, 0:n], bi == 0, bi == nb - 1),
                         r=vk + [("PTs", bi % 3)], w=[("ps", 6)])
                    accL, ak = (rsb[:], ("rsb",)) if bi % 2 == 0 else (t1, ("t1",))
                    if bi < 2:
                        P.dve(cp(accL, pt[:, 0:n]), r=[("PTs", bi % 3), ak] + REG_A, w=[ak])
                    else:
                        P.dve(tt(accL[:, 128 * t:GS], accL[:, 128 * t:GS], pt[:, 0:n], ALU.add),
                              r=[("PTs", bi % 3), ak], w=[ak])

                emit_S(0)
                emit_S(1)
                for bi in range(nb):
                    if bi + 2 < nb:
                        emit_S(bi + 2)
                    emit_PV(bi)
                tok = slice(qtok0, qtok0 + GS)
                P.dve(tt(rsb[:], rsb[:], t1, ALU.add), r=[("rsb",), ("t1",)], w=[("rsb",)])
                P.pe(mm(PS[7][:], onesf[:], rsb[:], True, True), r=[("cbf",), ("rsb",)], w=[("ps", 7)])
                P.dve(lambda e: e.reciprocal(t1, PS[7][:]), r=[("ps", 7)] + REG_A, w=[("t1",)])
                P.dve(tt(t1, PS[6][:], t1, ALU.mult), r=[("ps", 6), ("t1",)], w=[("t1",)])
                P.pool(tt(sgh[:, tok], t1, sgh[:, tok], ALU.mult), r=[("t1",), ("sgh", qg)], w=[("sgh", qg)])
            if h == 0:
                dbg("og", sgh, [("sgh", g) for g in range(NG)])
            P.dma("sp", dmaf(mixd[h], sgh), r=[("sgh", g) for g in range(NG)], w=[("mixd", h)])

        outproj(l, 8, Wl["o"], C_POST + l * 8)
        barrier(REG_A)

    e_i = 0
    o_i = 0
    for l, kd in enumerate(layer_kinds):
        if kd == "e":
            even_layer(l, e_i, W[l])
            e_i += 1
        else:
            odd_layer(l, o_i, W[l])
            o_i += 1

    for q in range(4):
        P.dma("sp", dmaf(out_d[:, 2 * q:2 * q + 2, :], hT[:, 2 * q:2 * q + 2, :]), r=[("hT", 2 * q), ("hT", 2 * q + 1)],
              out=True)
    P.emit()
    return nc


C_EPS_ = 157


def _feat_major(a):
    t = a.shape[0]
    return np.ascontiguousarray(a.T.reshape(8, 128, t).transpose(1, 0, 2))


def _from_feat_major(a):
    t = a.shape[2]
    return np.ascontiguousarray(a.transpose(1, 0, 2).reshape(1024, t).T)


def _kmajor(w):
    k, c = w.shape
    return np.ascontiguousarray(w.reshape(k // 128, 128, c).transpose(1, 0, 2))


def _swap_cols(n_heads, d):
    idx = np.arange(n_heads * d).reshape(n_heads, 2, d // 2)
    return idx[:, ::-1, :].reshape(-1)


def _const_tables(core, layers, inp):
    cf = np.zeros((128, NCF), np.float32)
    cb = np.zeros((128, NCB), np.float32)
    p = np.arange(128)
    e_i = o_i = 0
    for li, l in enumerate(layers):
        cf[:, C_PRE + li * 8:C_PRE + li * 8 + 8] = inp["pre_norm"][l].reshape(8, 128).T
        cf[:, C_POST + li * 8:C_POST + li * 8 + 8] = inp["post_norm"][l].reshape(8, 128).T
        if l % 2 == 0:
            e = l // 2
            for tap in range(3):
                cf[:, C_CW + e_i * 24 + tap * 8:C_CW + e_i * 24 + tap * 8 + 8] = inp["even_conv_w"][e][tap].reshape(8, 128).T
            cf[:, C_CB + e_i * 8:C_CB + e_i * 8 + 8] = inp["even_conv_b"][e].reshape(8, 128).T
            cf[:, C_GN + e_i * 8:C_GN + e_i * 8 + 8] = inp["ret_gn"][e].reshape(8, 128).T
            e_i += 1
        else:
            o = l // 2
            cf[:, C_QAN + o_i * 3:C_QAN + o_i * 3 + 3] = inp["q_a_norm"][o].reshape(3, 128).T
            cf[:, C_KVAN + o_i * 2:C_KVAN + o_i * 2 + 2] = inp["kv_a_norm"][o].reshape(2, 128).T
            o_i += 1
    is_b = float(core % 2)
    inv = (10000.0 ** (-np.arange(0, 64, 2, dtype=np.float32) / np.float32(64))).astype(np.float32)
    cf[:, C_INVF] = inv[p % 32]
    cf[:, C_SGN] = np.where((p % 64) < 32, -1.0, 1.0)
    cf[:, C_ISB] = is_b
    cf[:, C_EPS_] = EPS
    gam = 1.0 - 2.0 ** (-5.0 - np.arange(8, dtype=np.float64))
    i = np.arange(128, dtype=np.float64)
    for hp in range(4):
        gh = gam[2 * hp + (p // 64)]
        cf[:, C_G128 + hp] = gh ** 128
        for n in range(16):
            cf[:, C_CFIX + hp * 16 + n] = is_b * gh ** (128 * n)
        cb[:, CB_DQ + hp * 128:CB_DQ + (hp + 1) * 128] = gh[:, None] ** i[None, :]
        cb[:, CB_DK + hp * 128:CB_DK + (hp + 1) * 128] = 0.125 * gh[:, None] ** (-i[None, :])
    jj = np.arange(128)[:, None]
    ii = np.arange(128)[None, :]
    cb[:, CB_MASK:CB_MASK + 128] = (ii >= jj).astype(np.float32)
    cb[:, CB_TRI:CB_TRI + 128] = np.where(ii >= jj, 0.0, NEG)
    cb[:, CB_ID:CB_ID + 128] = np.eye(128, dtype=np.float32)
    return cf, cb


def _layer_weights(li, l, inp):
    d = {}
    if l % 2 == 0:
        e = l // 2
        w = inp["even_w_in"][e]
        sw = _swap_cols(8, 64)
        q, k = w[:, 0:512], w[:, 512:1024]
        v, g = w[:, 1024:2048], w[:, 2048:3072]
        qs, ks = q[:, sw], k[:, sw]
        r = np.empty((4, 128, 8, 1024), np.float32)
        for hp in range(4):
            c2 = slice(hp * 128, (hp + 1) * 128)
            c4 = slice(hp * 256, (hp + 1) * 256)
            blk = np.concatenate([k[:, c2], ks[:, c2], v[:, c4], q[:, c2], qs[:, c2], g[:, c4]], axis=1)
            r[hp] = _kmajor(blk)
        d["w%d_r" % li] = r
        c = np.empty((8, 128, 8, 512), np.float32)
        for i in range(8):
            cs = slice(i * 128, (i + 1) * 128)
            blk = np.concatenate([w[:, 3072:4096][:, cs], w[:, 4096:5120][:, cs], w[:, 5120:6144][:, cs],
                                  w[:, 6144:7168][:, cs]], axis=1)
            c[i] = _kmajor(blk)
        d["w%d_c" % li] = c
        wo = inp["even_w_out"][e]
        d["w%d_o" % li] = np.stack([_kmajor(wo[:, i * 128:(i + 1) * 128]) for i in range(8)])
    else:
        o = l // 2
        w = inp["odd_w_in"][o]
        sw = _swap_cols(1, 64)
        kr = w[:, 640:704]
        d["w%d_i" % li] = _kmajor(np.concatenate([w[:, 0:384], kr, kr[:, sw], w[:, 384:640]], axis=1))
        gate = w[:, 704:1728]
        d["w%d_g" % li] = np.stack([_kmajor(gate[:, h * 128:(h + 1) * 128]) for h in range(8)])
        wq = inp["w_qb"][o]
        wkv = inp["w_kvb"][o]
        qs_, kvs_ = [], []
        for h in range(8):
            b = h * 192
            rp = wq[:, b + 128:b + 192]
            qs_.append(_kmajor(np.concatenate([wq[:, b:b + 128], rp, rp[:, sw]], axis=1)))
            kvs_.append(_kmajor(wkv[:, h * 256:(h + 1) * 256]))
        d["w%d_q" % li] = np.stack(qs_)
        d["w%d_kv" % li] = np.stack(kvs_)
        wo = inp["odd_w_out"][o]
        d["w%d_o" % li] = np.stack([_kmajor(wo[:, i * 128:(i + 1) * 128]) for i in range(8)])
    return d


_PROG_CACHE = {}


def _run(layers, h_cores, inp, use_cc=True, debug=False):
    kinds = tuple("e" if l % 2 == 0 else "o" for l in layers)
    key = (kinds, use_cc, debug)
    if key not in _PROG_CACHE:
        _PROG_CACHE[key] = build_program(list(kinds), use_cc, debug)
    nc = _PROG_CACHE[key]
    shared = {}
    for li, l in enumerate(layers):
        shared.update(_layer_weights(li, l, inp))
    in_maps = []
    for c in range(8):
        b, half = c // 2, c % 2
        pos = inp["positions"][b, half * NT:(half + 1) * NT].astype(np.int32)
        mrow = np.zeros((1, 2 * NT), np.float32)
        mrow[0, :NT] = 1.0 - float(half)
        cf_, cb_ = _const_tables(c, layers, inp)
        m = {"xT": h_cores[c], "pos": np.ascontiguousarray(np.broadcast_to(pos[None, :], (128, NT))),
             "cf": cf_, "cb": cb_, "mrow": mrow}
        m.update(shared)
        in_maps.append(m)
    res = run_bass_kernel_spmd(nc, in_maps, core_ids=list(range(8)))
    if debug:
        return res
    return [np.asarray(res.results[c]["hT_out"]) for c in range(8)]


def kernel(x, positions, pre_norm, post_norm, even_w_in, even_conv_w, even_conv_b, ret_gn, even_w_out, odd_w_in,
           q_a_norm, w_qb, kv_a_norm, w_kvb, odd_w_out):
    inp = dict(x=np.asarray(x, np.float32), positions=np.asarray(positions), pre_norm=np.asarray(pre_norm, np.float32),
               post_norm=np.asarray(post_norm, np.float32), even_w_in=np.asarray(even_w_in, np.float32),
               even_conv_w=np.asarray(even_conv_w, np.float32), even_conv_b=np.asarray(even_conv_b, np.float32),
               ret_gn=np.asarray(ret_gn, np.float32), even_w_out=np.asarray(even_w_out, np.float32),
               odd_w_in=np.asarray(odd_w_in, np.float32), q_a_norm=np.asarray(q_a_norm, np.float32),
               w_qb=np.asarray(w_qb, np.float32), kv_a_norm=np.asarray(kv_a_norm, np.float32),
               w_kvb=np.asarray(w_kvb, np.float32), odd_w_out=np.asarray(odd_w_out, np.float32))
    h = [_feat_major(inp["x"][c // 2, (c % 2) * NT:(c % 2 + 1) * NT, :]) for c in range(8)]
    h = _run(LAUNCHES[0], h, inp) if len(LAUNCHES) == 1 else _multi(h, inp)
    out = np.empty((4, 4096, 1024), np.float32)
    for c in range(8):
        out[c // 2, (c % 2) * NT:(c % 2 + 1) * NT, :] = _from_feat_major(h[c])
    return out


def _multi(h, inp):
    for ls in LAUNCHES:
        h = _run(ls, h, inp)
    return h


LAUNCHES = [[0, 1, 2, 3]]
```
